# Optimizing a Trainium2 kernel written in Bass

```python
import math
import jax, jax.numpy as jnp
from jax import lax
import numpy as np

D_MODEL = 1024
BATCH = 4
SEQ = 4096
DEPTH = 2

N_A = DEPTH // 2
N_B = DEPTH - N_A

GLA_HEADS = 4
GLA_DK = (D_MODEL // 2) // GLA_HEADS
GLA_DV = D_MODEL // GLA_HEADS
GLA_GATE_RANK = 16
GLA_TAU = 16.0
GLA_CHUNK = 64
GLA_COLS = (GLA_HEADS * GLA_DK, GLA_HEADS * GLA_DK, GLA_HEADS * GLA_DV, GLA_HEADS * GLA_DV, GLA_GATE_RANK)
GLA_SPLITS = list(np.cumsum(GLA_COLS)[:-1].tolist())
GLA_IN = int(sum(GLA_COLS))

DIFF_HEADS = 8
DIFF_DH = 64
DIFF_VD = 2 * DIFF_DH
Q_BLOCK = 128

MLP_HIDDEN = 4 * D_MODEL
NORM_EPS = 1e-6

kernel_name = "yoco_gla_diffattn_hybrid"


def rms_norm(x, g):
    xf = x.astype(jnp.float32)
    y = xf * lax.rsqrt(jnp.mean(xf * xf, axis=-1, keepdims=True) + NORM_EPS)
    return (y * g.astype(jnp.float32)).astype(x.dtype)


def sqrelu_mlp(xn, w1, w2):
    return jnp.square(jax.nn.relu(xn @ w1)) @ w2


def gla_chunked(q, k, v, log_a):
    B, S, H, DK = q.shape
    DV = v.shape[-1]
    C = GLA_CHUNK
    n = S // C

    def chunks(t):
        return t.astype(jnp.float32).reshape(B, n, C, H, t.shape[-1]).transpose(1, 0, 3, 2, 4)

    qc = chunks(q) * (DK ** -0.5)
    kc = chunks(k)
    vc = chunks(v)
    bc = jnp.cumsum(chunks(log_a), axis=-2)
    causal = jnp.tril(jnp.ones((C, C), bool))

    def step(state, inp):
        qi, ki, vi, bi = inp
        diff = bi[:, :, :, None, :] - bi[:, :, None, :, :]
        decay = jnp.exp(jnp.where(causal[:, :, None], diff, -jnp.inf))
        attn = jnp.einsum('bhid,bhjd,bhijd->bhij', qi, ki, decay)
        o = (jnp.einsum('bhij,bhjv->bhiv', attn, vi)
             + jnp.einsum('bhid,bhdv->bhiv', qi * jnp.exp(bi), state))
        b_last = bi[:, :, -1:, :]
        state = (jnp.exp(b_last[:, :, 0, :, None]) * state
                 + jnp.einsum('bhjd,bhjv->bhdv', ki * jnp.exp(b_last - bi), vi))
        return state, o

    _, o = lax.scan(step, jnp.zeros((B, H, DK, DV), jnp.float32), (qc, kc, vc, bc))
    return o.transpose(1, 0, 3, 2, 4).reshape(B, S, H, DV)


def gla_mixer(xn, w_in, w_gate_up, b_gate, head_gain, w_out):
    B, S, _ = xn.shape
    proj = xn @ w_in
    q, k, v, g, z = jnp.split(proj, GLA_SPLITS, axis=-1)
    log_a = jax.nn.log_sigmoid((z @ w_gate_up + b_gate).astype(jnp.float32)) / GLA_TAU
    hk = lambda t: t.reshape(B, S, GLA_HEADS, GLA_DK)
    o = gla_chunked(hk(q), hk(k), v.reshape(B, S, GLA_HEADS, GLA_DV), hk(log_a)).astype(xn.dtype)
    o = rms_norm(o, head_gain) * jax.nn.silu(g.reshape(B, S, GLA_HEADS, GLA_DV))
    return o.reshape(B, S, GLA_HEADS * GLA_DV) @ w_out


def shared_kv(h, kv_norm, w_kv, k_norm):
    B, S, _ = h.shape
    kv = rms_norm(h, kv_norm) @ w_kv
    k, v = jnp.split(kv, [DIFF_HEADS * 2 * DIFF_DH], axis=-1)
    k = rms_norm(k.reshape(B, S, DIFF_HEADS, 2, DIFF_DH), k_norm).transpose(0, 2, 3, 1, 4)
    v = v.reshape(B, S, DIFF_HEADS, DIFF_VD).transpose(0, 2, 1, 3)
    return k, v


def diff_mixer(xn, k, v, w_q, q_gain, lam_params, head_gain, w_out, lambda_init):
    B, S, _ = xn.shape
    q = rms_norm((xn @ w_q).reshape(B, S, DIFF_HEADS, 2, DIFF_DH), q_gain) * (DIFF_DH ** -0.5)
    lp = lam_params.astype(jnp.float32)
    lam = jnp.exp(jnp.sum(lp[0] * lp[1])) - jnp.exp(jnp.sum(lp[2] * lp[3])) + lambda_init
    nq = S // Q_BLOCK
    qb = q.transpose(0, 2, 3, 1, 4).reshape(B, DIFF_HEADS, 2, nq, Q_BLOCK, DIFF_DH).transpose(3, 0, 1, 2, 4, 5)
    kpos = jnp.arange(S)

    def attend(args):
        q_blk, bi = args
        s = jnp.einsum('bhcqd,bhckd->bhcqk', q_blk, k).astype(jnp.float32)
        qpos = bi * Q_BLOCK + jnp.arange(Q_BLOCK)
        s = jnp.where(kpos[None, :] <= qpos[:, None], s, -jnp.inf)
        p = jax.nn.softmax(s, axis=-1)
        a = p[:, :, 0] - lam * p[:, :, 1]
        return jnp.einsum('bhqk,bhkv->bhqv', a.astype(v.dtype), v)

    o = lax.map(attend, (qb, jnp.arange(nq)))
    o = o.transpose(1, 0, 3, 2, 4).reshape(B, S, DIFF_HEADS, DIFF_VD)
    o = rms_norm(o, head_gain) * (1.0 - lambda_init)
    return o.reshape(B, S, DIFF_HEADS * DIFF_VD) @ w_out


def setup_inputs(seed: int = 0) -> dict:
    key = jax.random.key(seed)
    ks = jax.random.split(key, 24)
    f32 = jnp.float32
    nrm = lambda k, shape, fan_in: jax.random.normal(k, shape, f32) * (fan_in ** -0.5)
    gain = lambda k, shape: 1.0 + 0.02 * jax.random.normal(k, shape, f32)
    D = D_MODEL
    return {
        "x": jax.random.normal(ks[0], (BATCH, SEQ, D), f32),
        "a_norm": gain(ks[1], (N_A, D)),
        "a_w_in": nrm(ks[2], (N_A, D, GLA_IN), D),
        "a_w_gate_up": nrm(ks[3], (N_A, GLA_GATE_RANK, GLA_HEADS * GLA_DK), GLA_GATE_RANK),
        "a_b_gate": 0.1 * jax.random.normal(ks[4], (N_A, GLA_HEADS * GLA_DK), f32),
        "a_head_norm": gain(ks[5], (N_A, GLA_DV)),
        "a_w_out": nrm(ks[6], (N_A, GLA_HEADS * GLA_DV, D), GLA_HEADS * GLA_DV),
        "kv_norm": gain(ks[7], (D,)),
        "w_kv": nrm(ks[8], (D, DIFF_HEADS * (2 * DIFF_DH + DIFF_VD)), D),
        "k_norm": gain(ks[9], (DIFF_DH,)),
        "b_norm": gain(ks[10], (N_B, D)),
        "b_w_q": nrm(ks[11], (N_B, D, DIFF_HEADS * 2 * DIFF_DH), D),
        "b_q_norm": gain(ks[12], (N_B, DIFF_DH)),
        "b_lambda": 0.1 * jax.random.normal(ks[13], (N_B, 4, DIFF_DH), f32),
        "b_head_norm": gain(ks[14], (N_B, DIFF_VD)),
        "b_w_out": nrm(ks[15], (N_B, DIFF_HEADS * DIFF_VD, D), DIFF_HEADS * DIFF_VD),
        "mlp_norm": gain(ks[16], (DEPTH, D)),
        "mlp_w1": nrm(ks[17], (DEPTH, D, MLP_HIDDEN), D),
        "mlp_w2": nrm(ks[18], (DEPTH, MLP_HIDDEN, D), MLP_HIDDEN),
    }


def reference(x, a_norm, a_w_in, a_w_gate_up, a_b_gate, a_head_norm, a_w_out,
              kv_norm, w_kv, k_norm, b_norm, b_w_q, b_q_norm, b_lambda, b_head_norm, b_w_out,
              mlp_norm, mlp_w1, mlp_w2):
    h = x
    k_sh, v_sh = None, None
    for layer in range(DEPTH):
        if layer < N_A:
            i = layer
            h = h + gla_mixer(rms_norm(h, a_norm[i]), a_w_in[i], a_w_gate_up[i], a_b_gate[i],
                              a_head_norm[i], a_w_out[i])
        else:
            if layer == N_A:
                k_sh, v_sh = shared_kv(h, kv_norm, w_kv, k_norm)
            j = layer - N_A
            lambda_init = 0.8 - 0.6 * math.exp(-0.3 * layer)
            h = h + diff_mixer(rms_norm(h, b_norm[j]), k_sh, v_sh, b_w_q[j], b_q_norm[j],
                               b_lambda[j], b_head_norm[j], b_w_out[j], lambda_init)
        h = h + sqrelu_mlp(rms_norm(h, mlp_norm[layer]), mlp_w1[layer], mlp_w2[layer])
    return h
```

```python
import math
import numpy as np
import concourse.bass as bass
import concourse.mybir as mybir
from concourse.bass_utils import run_bass_kernel_spmd

F32 = mybir.dt.float32
BF16 = mybir.dt.bfloat16
ALU = mybir.AluOpType
AF = mybir.ActivationFunctionType

ENGINES = ("pe", "act", "dve", "pool", "sp")
SELF_SYNC = {"pe": False, "act": True, "dve": True, "pool": True, "sp": False}
MARKS_PER_SEM = 30000


class Buf:
    __slots__ = ("name", "last_w", "readers")

    def __init__(self, name):
        self.name = name
        self.last_w = None
        self.readers = []


class Op:
    __slots__ = ("idx", "eng", "emit", "deps", "dma_key", "dma_cnt", "marked",
                 "mark_sem", "mark_val", "eidx")


class Prog:
    def __init__(self, nc):
        self.nc = nc
        self.ops = []
        self.eng_ops = {e: [] for e in ENGINES}
        self.dma_counts = {}
        self.bufs = {}

    def buf(self, name):
        b = self.bufs.get(name)
        if b is None:
            b = Buf(name)
            self.bufs[name] = b
        return b

    def _mk(self, eng, emit, reads, writes, dma_key=None):
        op = Op()
        op.idx = len(self.ops)
        op.eng = eng
        op.emit = emit
        op.dma_key = dma_key
        op.marked = False
        op.eidx = len(self.eng_ops[eng])
        deps = set()
        rb = [self.buf(r) for r in reads]
        wb = [self.buf(w) for w in writes]
        for b in rb:
            if b.last_w is not None:
                deps.add(b.last_w)
        for b in wb:
            if b.last_w is not None:
                deps.add(b.last_w)
            deps.update(b.readers)
        op.deps = deps
        if dma_key is not None:
            self.dma_counts[dma_key] = self.dma_counts.get(dma_key, 0) + 1
            op.dma_cnt = self.dma_counts[dma_key]
        for b in rb:
            b.readers.append(op.idx)
        for b in wb:
            b.last_w = op.idx
            b.readers = []
        self.ops.append(op)
        self.eng_ops[eng].append(op)
        return op

    def op(self, eng, emit, reads=(), writes=()):
        return self._mk(eng, emit, reads, writes)

    def dma(self, eng, key, emit, reads=(), writes=()):
        return self._mk(eng, emit, reads, writes, dma_key=key)

    def build(self, final_dma_keys=()):
        nc = self.nc
        ops = self.ops
        seen = {e: {p: -1 for p in ENGINES} for e in ENGINES}
        seen_dma = {e: {} for e in ENGINES}
        waits = [None] * len(ops)
        dma_issued = {}
        for op in ops:
            w_eng = {}
            w_dma = {}
            for d in op.deps:
                p = ops[d]
                if p.dma_key is not None:
                    cnt = dma_issued[p.dma_key]
                    if seen_dma[op.eng].get(p.dma_key, 0) < cnt:
                        w_dma[p.dma_key] = cnt
                else:
                    if p.eng == op.eng and not SELF_SYNC[op.eng]:
                        continue
                    if seen[op.eng][p.eng] < p.eidx:
                        if w_eng.get(p.eng, -1) < p.eidx:
                            w_eng[p.eng] = p.eidx
            for k, c in w_dma.items():
                seen_dma[op.eng][k] = c
            for pe_, ei in w_eng.items():
                seen[op.eng][pe_] = ei
                self.eng_ops[pe_][ei].marked = True
            waits[op.idx] = (w_eng, w_dma)
            if op.dma_key is not None:
                dma_issued[op.dma_key] = op.dma_cnt
        for e in ENGINES:
            n = 0
            lst = []
            for op in self.eng_ops[e]:
                if op.dma_key is None and op.marked:
                    si = n // MARKS_PER_SEM
                    if si >= len(lst):
                        lst.append(nc.alloc_semaphore(f"m_{e}_{si}"))
                    op.mark_sem = lst[si]
                    op.mark_val = n % MARKS_PER_SEM + 1
                    n += 1
        dma_sems = {k: nc.alloc_semaphore(f"d_{k}") for k in self.dma_counts}
        self.n_waits = 0
        eng_objs = {"pe": nc.tensor, "act": nc.scalar, "dve": nc.vector,
                    "pool": nc.gpsimd, "sp": nc.sync}

        def run_engine(e):
            eo = eng_objs[e]
            for op in self.eng_ops[e]:
                w_eng, w_dma = waits[op.idx]
                for pe_, ei in w_eng.items():
                    pop = self.eng_ops[pe_][ei]
                    eo.wait_ge(pop.mark_sem, pop.mark_val)
                    self.n_waits += 1
                for k, c in w_dma.items():
                    eo.wait_ge(dma_sems[k], 16 * c)
                    self.n_waits += 1
                ins = op.emit(eo)
                if op.dma_key is not None:
                    ins.then_inc(dma_sems[op.dma_key], 16)
                elif op.marked:
                    ins.then_inc(op.mark_sem, 1)
            if e == "sp":
                for k in final_dma_keys:
                    eo.wait_ge(dma_sems[k], 16 * self.dma_counts[k])

        with nc.Block() as block:
            @block.tensor
            def _(eng):
                run_engine("pe")

            @block.scalar
            def _(eng):
                run_engine("act")

            @block.vector
            def _(eng):
                run_engine("dve")

            @block.gpsimd
            def _(eng):
                run_engine("pool")

            @block.sync
            def _(eng):
                run_engine("sp")


D = 1024
KC = 8
G = 512
NCH = 4
GLA_IN = 3088
TAU = 16.0
EPS = 1e-6
LAMBDA_INIT = 0.8 - 0.6 * math.exp(-0.3 * 1)
RING = 5

C_ANORM, C_MLP0, C_KVN, C_BN, C_MLP1 = 0, 8, 16, 24, 32
C_AHEAD = 40
C_KN, C_QN, C_BHEAD, C_PREF = 42, 43, 44, 45
C_LAM = 46
C_BGATE = 302
C_KNB = 814
C_QNB = 878
NSMALL = 942


def N(prefix, it):
    return [f"{prefix}{i}" for i in it]


def build_program(S_ALL, S_OWN, dbg=False):
    NPRE = S_ALL - S_OWN
    NG_ALL = S_ALL // G
    NG_OWN = S_OWN // G
    G_OWN0 = NPRE // G
    nc = bass.Bass("TRN2", target_bir_lowering=False)

    def din(name, shape, dt=F32):
        return nc.dram_tensor(name, list(shape), dt, kind="ExternalInput").ap()

    xT = din("xT", [128, KC, S_ALL])
    smallp_d = din("smallp", [128, NSMALL])
    consts_d = din("consts", [128, 512])
    a_w_in = din("a_w_in", [D, GLA_IN])
    a_w_gate_up = din("a_w_gate_up", [16, 512])
    a_w_out = din("a_w_out", [D, D])
    w_kv = din("w_kv", [D, 2048])
    b_w_q = din("b_w_q", [D, D])
    b_w_out = din("b_w_out", [D, D])
    mlp_w1 = din("mlp_w1", [2, D, 4096])
    mlp_w2 = din("mlp_w2", [2, 4096, D])
    outT = nc.dram_tensor("outT", [128, KC, S_OWN], F32, kind="ExternalOutput").ap()

    skind = "ExternalOutput" if dbg else "Internal"
    NBLK_MAX = 48
    wstream = nc.dram_tensor("wstream", [NBLK_MAX, 128, 4096], BF16).ap()
    KTs = nc.dram_tensor("KTs", [8, 128, S_ALL], BF16, kind=skind).ap()
    Vs = nc.dram_tensor("Vs", [8, 128, S_ALL // 128, 128], BF16, kind=skind).ap()
    QTs = nc.dram_tensor("QTs", [8, 128, S_OWN], BF16, kind=skind).ap()
    H1s = nc.dram_tensor("H1s", [128, KC, S_OWN], F32, kind=skind).ap()
    OAs = nc.dram_tensor("OAs", [8, 128, S_OWN], BF16, kind=skind).ap()

    sb = nc.alloc_sbuf_tensor
    ring = [sb(f"ring{i}", [128, 8, 512], BF16) for i in range(RING)]
    hT = sb("hT", [128, KC, G], F32)
    xn = sb("xn", [128, KC, G], BF16)
    sq = sb("sq", [128, KC, G], BF16)
    big = sb("big", [128, 32, G], BF16)
    qs = sb("qs", [128, 4, G], BF16)
    ks = sb("ks", [128, 4, G], BF16)
    ktok = sb("ktok", [128, NCH, 512], BF16)
    vtok = sb("vtok", [128, NCH, 1024], BF16)
    sg = sb("sg", [128, KC, G], BF16)
    eq = sb("eq", [128, 4, G], BF16)
    ek = sb("ek", [128, 4, G], BF16)
    lg = sb("lg", [128, 512], F32)
    ogt = sb("ogt", [128, 8, 128], F32)
    sp_t = sb("sp", [128, 512], BF16)
    AT = sb("AT", [128, NCH, 4, 128], BF16)
    U = sb("U", [128, 4, 256], F32)
    Sbf = sb("Sbf", [128, NCH + 1, 4, 256], BF16)
    elast = sb("elast", [128, 4], F32)
    ecur = sb("ecur", [128, NCH, 4], F32)
    rstd = sb("rstd", [128, G], F32)
    rstd2 = sb("rstd2", [128, G], F32)
    xg = sb("xg", [128, KC, G], BF16)
    QTz = sb("QTz", [128, 2, 2, S_OWN], BF16)
    zT = sb("zT", [33, G], BF16)
    wz = sb("wz", [128, KC, 16], BF16)
    wg = sb("wg", [33, 512], BF16)
    smallp = sb("smallp_sb", [128, NSMALL], F32)
    cf = sb("cf", [128, 512], F32)
    cb = sb("cb", [128, 512], BF16)
    derived = sb("derived", [128, 16], F32)
    lamtmp = sb("lamtmp", [128, 128], F32)
    ident = cb[:, 0:128]
    tri = cb[:, 128:256]
    ones = cb[:, 256:384]
    bones = cb[:, 384:512]
    mask4 = sb("mask4", [128, 4, 128], BF16)
    pall = nc.alloc_psum_tensor("pall", [128, 8, 512], F32)
    psum = [pall[:, i, :] for i in range(8)]
    print("sbuf bytes remaining", nc.sbuf_bytes_remaining)

    DV_QG, DV_HG1, DV_NLAM, DV_BALL, DV_BPRE, DV_T0, DV_T1, DV_T2 = range(8)

    def gen(P, stream_plan):
        st = {"ps": 0, "blk": 0, "gla": 0}
        specs = []

        def PS():
            i = st["ps"] % 8
            st["ps"] += 1
            return psum[i], f"ps{i}"

        def issue_load(i):
            if stream_plan is None or i >= len(stream_plan):
                return
            slot = i % RING
            bid = stream_plan[i][1]
            P.dma("sp", f"ring{slot}",
                  lambda e, slot=slot, bid=bid: e.dma_start(
                      out=ring[slot][:].rearrange("p a b -> p (a b)"), in_=wstream[bid]),
                  reads=[f"wblk{bid}"], writes=[f"ring{slot}"])

        def next_block(spec):
            i = st["blk"]
            st["blk"] += 1
            specs.append(spec)
            if stream_plan is not None:
                assert stream_plan[i][0] == spec, (stream_plan[i], spec)
                issue_load(i + RING - 1)
            slot = i % RING
            return ring[slot], f"ring{slot}"

        def xload(g):
            t0 = g * G
            P.dma("pool", "xin", lambda e: e.dma_start(out=hT[:], in_=xT[:, :, t0:t0 + G]),
                  writes=N("hT", range(8)))

        xload(0)
        P.op("pool", lambda e: e.memset(U[:], 0.0), writes=N("U", range(4)))
        P.op("pool", lambda e: e.memset(zT[:], 0.0), writes=["zT"])
        P.op("pool", lambda e: e.memset(zT[32:33, :], 1.0), writes=["zT"])
        P.op("pool", lambda e: e.memset(wg[:], 0.0), writes=["wg"])
        P.op("pool", lambda e: e.memset(QTz[:, 0], 0.0), writes=["QTz0"])
        P.op("pool", lambda e: e.memset(QTz[:, 1], 0.0), writes=["QTz1"])
        P.op("pool", lambda e: e.memset(Sbf[:], 0.0), writes=[f"Sbf{i}_{h}" for i in range(NCH + 1) for h in range(4)])
        P.op("pool", lambda e: e.memset(elast[:], 1.0), writes=["elast"])
        if stream_plan is not None:
            P.dma("sp", "small", lambda e: e.dma_start(out=smallp[:], in_=smallp_d), writes=["smallp"])
            P.dma("sp", "small", lambda e: e.dma_start(out=cf[:], in_=consts_d), writes=["cf"])
            P.op("act", lambda e: e.activation(out=cb[:], in_=cf[:], func=AF.Copy), reads=["cf"], writes=["cb"])
            P.op("pool", lambda e: e.tensor_copy(out=mask4[:, 0, :], in_=cf[:, 128:256]), reads=["cf"], writes=["mask4"])
            for h in range(1, 4):
                P.op("pool", lambda e, h=h: e.tensor_copy(out=mask4[:, h, :], in_=cf[:, 128:256]), reads=["cf"], writes=["mask4"])

            P.dma("pool", "wz", lambda e: e.dma_start(
                out=wz[:], in_=a_w_in[:, 3072:3088].rearrange("(kc p) c -> p kc c", p=128)), writes=["wz"])
            P.dma("pool", "wz", lambda e: e.dma_start(out=wg[0:16, :], in_=a_w_gate_up), writes=["wg"])
            P.dma("pool", "wz", lambda e: e.dma_start(out=wg[32:33, :], in_=smallp_d[0:1, C_BGATE:C_BGATE + 512]), writes=["wg"])
            seen_b = set()
            for spec, bid in stream_plan:
                if bid in seen_b:
                    continue
                seen_b.add(bid)
                wname, r0, c0 = spec
                src = {"a_w_in": a_w_in, "a_w_out": a_w_out, "w_kv": w_kv, "b_w_q": b_w_q,
                       "b_w_out": b_w_out, "w1_0": mlp_w1[0], "w1_1": mlp_w1[1],
                       "w2_0": mlp_w2[0], "w2_1": mlp_w2[1]}[wname]
                sap = src[r0:r0 + 1024, c0:c0 + 512].rearrange("(kc p) c -> p kc c", p=128)
                dap = wstream[bid].rearrange("p (kc c) -> p kc c", kc=8)
                P.dma("pool", f"cast{bid}", lambda e, sap=sap, dap=dap: e.dma_start(out=dap, in_=sap),
                      writes=[f"wblk{bid}"])
            for i in range(RING - 1):
                issue_load(i)
        dcol = lambda i: derived[:, i:i + 1]
        P.op("dve", lambda e: e.tensor_scalar(out=dcol(DV_QG), in0=smallp[:, C_QN:C_QN + 1], scalar1=0.125,
                                              scalar2=None, op0=ALU.mult), reads=["smallp"], writes=["derived"])
        P.op("dve", lambda e: e.tensor_scalar(out=dcol(DV_HG1), in0=smallp[:, C_BHEAD:C_BHEAD + 1],
                                              scalar1=1.0 - LAMBDA_INIT, scalar2=None, op0=ALU.mult),
             reads=["smallp"], writes=["derived"])
        P.op("dve", lambda e: e.tensor_tensor(out=lamtmp[:, 0:64], in0=smallp[:, C_LAM:C_LAM + 64],
                                              in1=smallp[:, C_LAM + 64:C_LAM + 128], op=ALU.mult),
             reads=["smallp"], writes=["lamtmp"])
        P.op("dve", lambda e: e.tensor_tensor(out=lamtmp[:, 64:128], in0=smallp[:, C_LAM + 128:C_LAM + 192],
                                              in1=smallp[:, C_LAM + 192:C_LAM + 256], op=ALU.mult),
             reads=["smallp", "lamtmp"], writes=["lamtmp"])
        P.op("dve", lambda e: e.reduce_sum(out=dcol(DV_T0), in_=lamtmp[:, 0:64], axis=mybir.AxisListType.X),
             reads=["lamtmp", "derived"], writes=["derived"])
        P.op("dve", lambda e: e.reduce_sum(out=dcol(DV_T1), in_=lamtmp[:, 64:128], axis=mybir.AxisListType.X),
             reads=["lamtmp", "derived"], writes=["derived"])
        P.op("act", lambda e: e.activation(out=derived[:, DV_T0:DV_T1 + 1], in_=derived[:, DV_T0:DV_T1 + 1], func=AF.Exp),
             reads=["derived"], writes=["derived"])
        P.op("dve", lambda e: e.scalar_tensor_tensor(out=dcol(DV_NLAM), in0=dcol(DV_T1), scalar=-LAMBDA_INIT,
                                                     in1=dcol(DV_T0), op0=ALU.add, op1=ALU.subtract),
             reads=["derived"], writes=["derived"])
        P.op("dve", lambda e: e.tensor_reduce(out=dcol(DV_T0), in_=smallp[:, C_KNB:C_KNB + 64], axis=mybir.AxisListType.X,
                                              op=ALU.max, apply_absolute_value=True),
             reads=["smallp", "derived"], writes=["derived"])
        P.op("dve", lambda e: e.tensor_reduce(out=dcol(DV_T1), in_=smallp[:, C_QNB:C_QNB + 64], axis=mybir.AxisListType.X,
                                              op=ALU.max, apply_absolute_value=True),
             reads=["smallp", "derived"], writes=["derived"])
        P.op("dve", lambda e: e.scalar_tensor_tensor(out=dcol(DV_BALL), in0=dcol(DV_T0), scalar=-8.0,
                                                     in1=dcol(DV_T1), op0=ALU.mult, op1=ALU.mult),
             reads=["derived"], writes=["derived"])
        P.op("dve", lambda e: e.tensor_tensor(out=dcol(DV_BPRE), in0=dcol(DV_BALL), in1=smallp[:, C_PREF:C_PREF + 1],
                                              op=ALU.add), reads=["derived", "smallp"], writes=["derived"])

        def norm_stage(gcol, tag, dst=None, dstn="xn"):
            dst = xn if dst is None else dst
            P.op("act", lambda e: e.activation(out=sq[:].rearrange("p a b -> p (a b)"),
                                               in_=hT[:].rearrange("p a b -> p (a b)"), func=AF.Square),
                 reads=N("hT", range(8)), writes=N("sq", range(8)))
            ps, pn = PS()
            for kc in range(KC):
                P.op("pe", lambda e, kc=kc, ps=ps: e.matmul(ps[:], lhsT=ones, rhs=sq[:, kc, :], start=(kc == 0), stop=(kc == KC - 1)),
                     reads=["cb", f"sq{kc}"], writes=[pn])
            P.op("act", lambda e, ps=ps: e.activation(out=rstd[:], in_=ps[:], func=AF.Ln, scale=1.0 / D, bias=EPS),
                 reads=[pn], writes=["rstd"])
            P.op("act", lambda e: e.activation(out=rstd[:], in_=rstd[:], func=AF.Exp, scale=-0.5),
                 reads=["rstd"], writes=["rstd"])
            for kc in range(KC):
                eng = "dve"
                P.op(eng, lambda e, kc=kc: e.scalar_tensor_tensor(
                    out=dst[:, kc, :], in0=hT[:, kc, :], scalar=smallp[:, gcol + kc:gcol + kc + 1], in1=rstd[:],
                    op0=ALU.mult, op1=ALU.mult),
                     reads=[f"hT{kc}", "rstd", "smallp"], writes=[f"{dstn}{kc}"])

        def proj_fm(spec, nchunks, evac, rhs_t=None, rhs_names=None):
            rt = xn if rhs_t is None else rhs_t
            rn = "xn" if rhs_names is None else rhs_names
            W, wn = next_block(spec)
            for m in range(nchunks):
                ps, pn = PS()
                for kc in range(KC):
                    P.op("pe", lambda e, kc=kc, m=m, ps=ps, W=W: e.matmul(
                        ps[:], lhsT=W[:, kc, m * 128:(m + 1) * 128], rhs=rt[:, kc, :],
                        start=(kc == 0), stop=(kc == KC - 1)),
                         reads=[wn, f"{rn}{kc}"], writes=[pn])
                evac(m, ps, pn)

        def proj_tm(spec, dst, dstname, col0, src_t=None, src_n="xn"):
            W, wn = next_block(spec)
            srct = xn if src_t is None else src_t
            for c in range(NCH):
                ps, pn = PS()
                for kc in range(KC):
                    P.op("pe", lambda e, kc=kc, c=c, ps=ps, W=W: e.matmul(
                        ps[:], lhsT=srct[:, kc, c * 128:(c + 1) * 128], rhs=W[:, kc, :],
                        start=(kc == 0), stop=(kc == KC - 1)),
                         reads=[wn, f"{src_n}{kc}"], writes=[pn])
                P.op("act", lambda e, c=c, ps=ps: e.activation(out=dst[:, c, col0:col0 + 512], in_=ps[:], func=AF.Copy),
                     reads=[pn], writes=[f"{dstname}{c}"])

        def mlp_stage(layer, ncol):
            norm_stage(ncol, "mlp")
            for b in range(8):
                def evac(m, ps, pn, b=b):
                    c = b * 4 + m
                    P.op("act", lambda e, c=c, ps=ps: e.activation(out=big[:, c, :], in_=ps[:], func=AF.Relu),
                         reads=[pn], writes=[f"hid{c}"])
                    P.op("dve", lambda e, c=c: e.tensor_tensor(out=big[:, c, :], in0=big[:, c, :], in1=big[:, c, :], op=ALU.mult),
                         reads=[f"hid{c}"], writes=[f"hid{c}"])
                proj_fm((f"w1_{layer}", 0, b * 512), 4, evac)
            for half in range(2):
                acc = [PS() for _ in range(4)]
                for kcg in range(4):
                    W, wn = next_block((f"w2_{layer}", kcg * 1024, half * 512))
                    for mm in range(4):
                        ps, pn = acc[mm]
                        for kk in range(8):
                            c = kcg * 8 + kk
                            P.op("pe", lambda e, kk=kk, mm=mm, c=c, ps=ps, W=W: e.matmul(
                                ps[:], lhsT=W[:, kk, mm * 128:(mm + 1) * 128], rhs=big[:, c, :],
                                start=(c == 0), stop=(c == 31)),
                                 reads=[wn, f"hid{c}"], writes=[pn])
                for mm in range(4):
                    ps, pn = acc[mm]
                    m = half * 4 + mm
                    P.op("dve", lambda e, m=m, ps=ps: e.tensor_tensor(out=hT[:, m, :], in0=ps[:], in1=hT[:, m, :], op=ALU.add),
                         reads=[pn, f"hT{m}"], writes=[f"hT{m}"])

        def qk_norm_stage(wname, col0, gain_ap, dst_dram, t0, key, mid=None):
            pend = []

            def chain(h, ps, pn):
                ps2, pn2 = PS()
                P.op("pe", lambda e: e.matmul(ps2[:], lhsT=bones, rhs=sq[:, h, :], start=True, stop=True),
                     reads=["cb", f"sq{h}"], writes=[pn2])
                P.op("act", lambda e: e.activation(out=rstd2[:], in_=ps2[:], func=AF.Ln, scale=1.0 / 64, bias=EPS),
                     reads=[pn2], writes=["rstd2"])
                P.op("act", lambda e: e.activation(out=rstd2[:], in_=rstd2[:], func=AF.Exp, scale=-0.5),
                     reads=["rstd2"], writes=["rstd2"])
                P.op("dve", lambda e: e.scalar_tensor_tensor(
                    out=sg[:, h, :], in0=ps[:], scalar=gain_ap, in1=rstd2[:], op0=ALU.mult, op1=ALU.mult),
                     reads=[pn, "rstd2", "smallp", "derived"], writes=[f"sg{h}"])

            for blk in range(2):
                def evac(m, ps, pn, blk=blk):
                    h = blk * 4 + m
                    P.op("act", lambda e, h=h, ps=ps: e.activation(out=sq[:, h, :], in_=ps[:], func=AF.Square),
                         reads=[pn], writes=[f"sq{h}"])
                    if pend:
                        chain(*pend.pop(0))
                    pend.append((h, ps, pn))
                proj_fm((wname, 0, col0 + blk * 512), 4, evac)
                if blk == 0 and mid is not None:
                    while pend:
                        chain(*pend.pop(0))
                    mid()
            while pend:
                chain(*pend.pop(0))
            P.dma("act", key, lambda e: e.dma_start(out=dst_dram.rearrange("h p t -> p h t")[:, :, t0:t0 + G], in_=sg[:]),
                  reads=N("sg", range(8)), writes=[key])

        def gla_norm():
            norm_stage(C_ANORM, "gla", dst=xg, dstn="xg")

        def gla_stage():
            ps, pn = PS()
            for kc in range(KC):
                P.op("pe", lambda e, kc=kc, ps=ps: e.matmul(ps[0:16, :], lhsT=wz[:, kc, :], rhs=xg[:, kc, :],
                                                              start=(kc == 0), stop=(kc == KC - 1)),
                     reads=["wz", f"xg{kc}"], writes=[pn])
            P.op("act", lambda e, ps=ps: e.activation(out=zT[0:16, :], in_=ps[0:16, :], func=AF.Copy), reads=[pn], writes=["zT"])
            for c in range(NCH):
                cs = slice(c * 128, (c + 1) * 128)
                ps, pn = PS()
                P.op("pe", lambda e, cs=cs, ps=ps: e.matmul(ps[:], lhsT=zT[:, cs], rhs=wg[:], start=True, stop=True),
                     reads=["zT", "wg"], writes=[pn])
                P.op("act", lambda e, ps=ps: e.activation(out=lg[:], in_=ps[:], func=AF.Exp, scale=-1.0), reads=[pn], writes=["lg"])
                P.op("act", lambda e: e.activation(out=sp_t[:], in_=lg[:], func=AF.Ln, bias=1.0), reads=["lg"], writes=["sp"])
                ps2, pn2 = PS()
                for h in range(4):
                    P.op("pe", lambda e, h=h, ps2=ps2: e.matmul(ps2[:, h * 128:(h + 1) * 128], lhsT=sp_t[:, h * 128:(h + 1) * 128],
                                                                 rhs=tri, start=True, stop=True),
                         reads=["sp", "cb"], writes=[pn2])
                psv = lambda p_: p_[:].rearrange("p (h i) -> p h i", h=4)
                P.op("act", lambda e, cs=cs, ps2=ps2: e.activation(out=eq[:, :, cs], in_=psv(ps2), func=AF.Exp, scale=-1.0 / TAU),
                     reads=[pn2], writes=[f"eq{c}"])
                P.op("act", lambda e, cs=cs, ps2=ps2: e.activation(out=ek[:, :, cs], in_=psv(ps2), func=AF.Exp, scale=1.0 / TAU),
                     reads=[pn2], writes=[f"ek{c}"])
            def evac_q(m, ps, pn):
                P.op("dve", lambda e, m=m, ps=ps: e.scalar_tensor_tensor(
                    out=qs[:, m, :], in0=ps[:], scalar=float(128 ** -0.5), in1=eq[:, m, :], op0=ALU.mult, op1=ALU.mult),
                     reads=[pn] + N("eq", range(4)), writes=[f"qs{m}"])
            proj_fm(("a_w_in", 0, 0), 4, evac_q, rhs_t=xg, rhs_names="xg")

            def evac_k(m, ps, pn):
                P.op("dve", lambda e, m=m, ps=ps: e.tensor_tensor(out=ks[:, m, :], in0=ps[:], in1=ek[:, m, :], op=ALU.mult),
                     reads=[pn] + N("ek", range(4)), writes=[f"ks{m}"])
            proj_fm(("a_w_in", 0, 512), 4, evac_k, rhs_t=xg, rhs_names="xg")
            proj_tm(("a_w_in", 0, 1024), vtok, "vtok", 0, src_t=xg, src_n="xg")
            proj_tm(("a_w_in", 0, 1536), vtok, "vtok", 512, src_t=xg, src_n="xg")
            for blk in range(2):
                def evac_g(m, ps, pn, blk=blk):
                    mm = blk * 4 + m
                    P.op("act", lambda e, mm=mm, ps=ps: e.activation(out=sg[:, mm, :], in_=ps[:], func=AF.Silu),
                         reads=[pn], writes=[f"sg{mm}"])
                proj_fm(("a_w_in", 0, 2048 + blk * 512), 4, evac_g, rhs_t=xg, rhs_names="xg")
            gidx = st["gla"]
            st["gla"] += 1
            CS = [slice(c * 128, (c + 1) * 128) for c in range(NCH)]
            for c in range(NCH):
                cs = CS[c]
                ps, pn = PS()
                for h in range(4):
                    P.op("pe", lambda e, h=h, cs=cs, ps=ps: e.matmul(ps[:, h * 128:(h + 1) * 128], lhsT=ks[:, h, cs], rhs=ident,
                                                                      start=True, stop=True),
                         reads=[f"ks{h}", "cb"], writes=[pn])
                P.op("act", lambda e, c=c, ps=ps: e.activation(out=ktok[:, c, :], in_=ps[:], func=AF.Copy),
                     reads=[pn], writes=[f"ktok{c}"])
                psA, pnA = PS()
                for h in range(4):
                    P.op("pe", lambda e, h=h, cs=cs, psA=psA: e.matmul(psA[:, h * 128:(h + 1) * 128], lhsT=ks[:, h, cs], rhs=qs[:, h, cs],
                                                                        start=True, stop=True),
                         reads=[f"ks{h}", f"qs{h}"], writes=[pnA])
                P.op("dve", lambda e, c=c, psA=psA: e.tensor_tensor(out=AT[:, c], in0=psA[:].rearrange("p (h i) -> p h i", h=4),
                                                                     in1=mask4[:], op=ALU.mult),
                     reads=[pnA, "mask4"], writes=[f"AT{c}"])
                P.op("act", lambda e, c=c: e.activation(out=ecur[:, c, :], in_=eq[:, :, c * 128 + 127], func=AF.Copy),
                     reads=[f"eq{c}"], writes=[f"ecur{c}"])
            for c in range(NCH):
                si_r = (gidx * NCH + c) % (NCH + 1)
                si_w = (gidx * NCH + c + 1) % (NCH + 1)
                psP = [PS(), PS()]
                for h in range(4):
                    ps_p, pn_p = psP[h // 2]
                    P.op("pe", lambda e, c=c, h=h, ps_p=ps_p: e.matmul(
                        ps_p[:, (h % 2) * 256:(h % 2 + 1) * 256], lhsT=ktok[:, c, h * 128:(h + 1) * 128],
                        rhs=vtok[:, c, h * 256:(h + 1) * 256], start=True, stop=True),
                         reads=[f"ktok{c}", f"vtok{c}"], writes=[pn_p])
                for h in range(4):
                    ps_p, pn_p = psP[h // 2]
                    ep = elast[:, h:h + 1] if c == 0 else ecur[:, c - 1, h:h + 1]
                    epn = "elast" if c == 0 else f"ecur{c - 1}"
                    P.op("dve", lambda e, h=h, ps_p=ps_p, ep=ep: e.scalar_tensor_tensor(
                        out=U[:, h, :], in0=U[:, h, :], scalar=ep, in1=ps_p[:, (h % 2) * 256:(h % 2 + 1) * 256],
                        op0=ALU.mult, op1=ALU.add),
                         reads=[f"U{h}", epn, pn_p], writes=[f"U{h}"])
                for h in range(4):
                    P.op("act", lambda e, h=h, c=c, si_w=si_w: e.activation(out=Sbf[:, si_w, h, :], in_=U[:, h, :], func=AF.Copy,
                                                                             scale=ecur[:, c, h:h + 1]),
                         reads=[f"U{h}", f"ecur{c}"], writes=[f"Sbf{si_w}_{h}"])
            P.op("dve", lambda e: e.tensor_copy(out=elast[:], in_=ecur[:, NCH - 1, :]), reads=[f"ecur{NCH - 1}"], writes=["elast"])

            psos = {}

            def emit_O(c):
                cs = CS[c]
                si_r = (gidx * NCH + c) % (NCH + 1)
                pso = [PS(), PS()]
                psos[c] = pso
                for h in range(4):
                    for vc in range(2):
                        ps_o, pn_o = pso[h // 2]
                        osl = slice(((h % 2) * 2 + vc) * 128, ((h % 2) * 2 + vc + 1) * 128)
                        vsl = slice(h * 256 + vc * 128, h * 256 + vc * 128 + 128)
                        P.op("pe", lambda e, c=c, h=h, osl=osl, vsl=vsl, ps_o=ps_o: e.matmul(
                            ps_o[:, osl], lhsT=vtok[:, c, vsl], rhs=AT[:, c, h, :], start=True, stop=False),
                             reads=[f"vtok{c}", f"AT{c}"], writes=[pn_o])
                        P.op("pe", lambda e, h=h, vc=vc, cs=cs, osl=osl, ps_o=ps_o, si_r=si_r: e.matmul(
                            ps_o[:, osl], lhsT=Sbf[:, si_r, h, vc * 128:(vc + 1) * 128], rhs=qs[:, h, cs], start=False, stop=True),
                             reads=[f"Sbf{si_r}_{h}", f"qs{h}"], writes=[pn_o])

            def emit_N(c):
                cs = CS[c]
                pso = psos.pop(c)
                psn, pnn = PS()
                for half in range(2):
                    ps_o, pn_o = pso[half]
                    P.op("act", lambda e, half=half, ps_o=ps_o: e.activation(out=sq[:, half * 4:half * 4 + 4, 0:128],
                                                                              in_=ps_o[:].rearrange("p (a i) -> p a i", a=4), func=AF.Square),
                         reads=[pn_o], writes=N("sq", range(half * 4, half * 4 + 4)))
                for h in range(4):
                    for vc in range(2):
                        P.op("pe", lambda e, h=h, vc=vc, psn=psn: e.matmul(psn[:, h * 128:(h + 1) * 128], lhsT=ones, rhs=sq[:, h * 2 + vc, 0:128],
                                                                            start=(vc == 0), stop=(vc == 1)),
                             reads=["cb", f"sq{h * 2 + vc}"], writes=[pnn])
                P.op("act", lambda e, psn=psn: e.activation(out=rstd2[:], in_=psn[:], func=AF.Ln, scale=1.0 / 256, bias=EPS),
                     reads=[pnn], writes=["rstd2"])
                P.op("act", lambda e: e.activation(out=rstd2[:], in_=rstd2[:], func=AF.Exp, scale=-0.5),
                     reads=["rstd2"], writes=["rstd2"])
                for h in range(4):
                    for vc in range(2):
                        m = h * 2 + vc
                        ps_o, pn_o = pso[h // 2]
                        osl = slice(((h % 2) * 2 + vc) * 128, ((h % 2) * 2 + vc + 1) * 128)
                        P.op("dve", lambda e, m=m, vc=vc, h=h, osl=osl, ps_o=ps_o: e.scalar_tensor_tensor(
                            out=ogt[:, m, :], in0=ps_o[:, osl], scalar=smallp[:, C_AHEAD + vc:C_AHEAD + vc + 1],
                            in1=rstd2[:, h * 128:(h + 1) * 128], op0=ALU.mult, op1=ALU.mult),
                             reads=[pn_o, "rstd2", "smallp"], writes=[f"ogt{m}"])
                        P.op("dve", lambda e, m=m, cs=cs: e.tensor_tensor(
                            out=xn[:, m, cs], in0=ogt[:, m, :], in1=sg[:, m, cs], op=ALU.mult),
                             reads=[f"ogt{m}", f"sg{m}"], writes=[f"xn{m}"])

            emit_O(0)
            for c in range(1, NCH):
                emit_O(c)
                emit_N(c - 1)
            emit_N(NCH - 1)
            for blk in range(2):
                def evac_o(m, ps, pn, blk=blk):
                    mm = blk * 4 + m
                    P.op("dve", lambda e, mm=mm, ps=ps: e.tensor_tensor(out=hT[:, mm, :], in0=ps[:], in1=hT[:, mm, :], op=ALU.add),
                         reads=[pn, f"hT{mm}"], writes=[f"hT{mm}"])
                proj_fm(("a_w_out", 0, blk * 512), 4, evac_o)

        def kv_stage(t0, after_norm=None, before_v=None):
            norm_stage(C_KVN, "kv")
            if after_norm is not None:
                after_norm()
            qk_norm_stage("w_kv", 0, smallp[:, C_KN:C_KN + 1], KTs, t0, "kst")
            if before_v is not None:
                before_v()
            proj_tm(("w_kv", 0, 1024), vtok, "vtok", 0)
            proj_tm(("w_kv", 0, 1536), vtok, "vtok", 512)
            blk0 = t0 // 128
            for c in range(NCH):
                P.dma("act", "vst", lambda e, c=c: e.dma_start(
                    out=Vs.rearrange("h p b v -> p b h v")[:, blk0 + c, :, :],
                    in_=vtok[:, c, :].rearrange("p (h v) -> p h v", h=8)),
                      reads=[f"vtok{c}"], writes=["Vs"])

        gla_norm()
        for g in range(NG_ALL):
            t0 = g * G
            gla_stage()
            mlp_stage(0, C_MLP0)
            has_next = g + 1 < NG_ALL
            nxt = (lambda g=g: xload(g + 1)) if has_next else None
            pre = gla_norm if has_next else None
            if g < G_OWN0:
                kv_stage(t0, after_norm=nxt, before_v=pre)
            else:
                kv_stage(t0)
                o0 = t0 - NPRE
                P.dma("act", "h1st", lambda e, o0=o0: e.dma_start(out=H1s[:, :, o0:o0 + G], in_=hT[:]),
                      reads=N("hT", range(8)), writes=["H1s"])
                for kc in range(KC):
                    P.op("dve", lambda e, kc=kc: e.scalar_tensor_tensor(
                        out=xn[:, kc, :], in0=hT[:, kc, :], scalar=smallp[:, C_BN + kc:C_BN + kc + 1], in1=rstd[:],
                        op0=ALU.mult, op1=ALU.mult),
                         reads=[f"hT{kc}", "rstd", "smallp"], writes=[f"xn{kc}"])
                if nxt is not None:
                    nxt()
                qk_norm_stage("b_w_q", 0, dcol(DV_QG), QTs, o0, "qst", mid=pre)

        KTb = [big[:, 0:8, :].rearrange("p a b -> p (a b)"), big[:, 8:16, :].rearrange("p a b -> p (a b)")]
        KTn = [N("hid", range(0, 8)), N("hid", range(8, 16))]
        Vb = [big[:, 16:24, :].rearrange("p a (b v) -> p (a b) v", v=128), big[:, 24:32, :].rearrange("p a (b v) -> p (a b) v", v=128)]
        Vn = [N("hid", range(16, 24)), N("hid", range(24, 32))]
        QTn = ["QTz0", "QTz1"]
        NKB_ALL = S_ALL // 128
        NPB = NPRE // 128
        LOOK = 2

        def load_head(h):
            b = h % 2
            P.dma("sp", f"ktl{b}", lambda e: e.dma_start(out=KTb[b][:, 0:S_ALL], in_=KTs[h]),
                  reads=["kst"], writes=KTn[b])
            P.dma("sp", f"vl{b}", lambda e: e.dma_start(out=Vb[b][:, 0:NKB_ALL, :], in_=Vs[h]),
                  reads=["Vs"], writes=Vn[b])
            P.dma("sp", f"ql{b}", lambda e: e.dma_start(out=QTz[0:64, b, 0, 0:S_OWN], in_=QTs[h][0:64, :]),
                  reads=["qst"], writes=[QTn[b]])
            P.dma("sp", f"ql{b}", lambda e: e.dma_start(out=QTz[64:128, b, 1, 0:S_OWN], in_=QTs[h][64:128, :]),
                  reads=["qst"], writes=[QTn[b]])

        ksteps = []
        for h in range(8):
            for qg in range(NG_OWN):
                nkb = NPB + (qg + 1) * 4
                for kb in range(nkb):
                    ksteps.append((h, qg, kb, nkb))
        rotp = [0]
        accO = [(psum[0], "ps0"), (psum[1], "ps1")]
        accL = [(psum[2], "ps2"), (psum[3], "ps3")]
        pts = {}

        def emit_qk(i):
            h, qg, kb, nkb = ksteps[i]
            b = h % 2
            q0 = qg * G
            dg = kb - (NPB + qg * 4)
            n0 = dg * 128 if dg > 0 else 0
            nn = G - n0
            k = rotp[0] % 2
            rotp[0] += 1
            base = 4 + 2 * k
            pp = pall[:, base:base + 2, :]
            ppn = [f"ps{base}", f"ps{base + 1}"]
            for c in range(2):
                P.op("pe", lambda e, c=c: e.matmul(pp[:, c, 0:nn], lhsT=KTb[b][:, kb * 128:(kb + 1) * 128],
                                                   rhs=QTz[:, b, c, q0 + n0:q0 + G], start=True, stop=True),
                     reads=KTn[b] + [QTn[b]], writes=[ppn[c]])
            pi = i % 4
            PT = sg[:, 2 * pi:2 * pi + 2, :]
            ptn = [f"sg{2 * pi}", f"sg{2 * pi + 1}"]
            bias_ap = dcol(DV_BPRE) if kb < NPB else dcol(DV_BALL)
            P.op("act", lambda e: e.activation(out=PT[:, :, 0:nn], in_=pp[:, :, 0:nn], func=AF.Exp, bias=bias_ap),
                 reads=ppn + ["derived"], writes=ptn)
            if dg >= 0:
                P.op("dve", lambda e: e.tensor_tensor(out=PT[:, :, 0:128], in0=PT[:, :, 0:128], in1=mask4[:, 0:2, :], op=ALU.mult),
                     reads=ptn + ["mask4"], writes=ptn)
            pts[i] = (PT, ptn, n0, nn)

        lpend = []
        lhold = {}

        def emit_L(PTs, ptn, n0, nn, first, last):
            for c in range(2):
                psL, pnL = accL[c]
                P.op("pe", lambda e, c=c, psL=psL: e.matmul(psL[:, n0:G], lhsT=ones, rhs=PTs[:, c, 0:nn], start=first, stop=last),
                     reads=["cb", ptn[c]], writes=[pnL])

        def emit_pv(i):
            h, qg, kb, nkb = ksteps[i]
            b = h % 2
            PT, ptn, n0, nn = pts.pop(i)
            ndiag0 = NPB + qg * 4
            while lpend:
                emit_L(*lpend.pop(0))
            for c in range(2):
                psO, pnO = accO[c]
                P.op("pe", lambda e, c=c, psO=psO: e.matmul(psO[:, n0:G], lhsT=Vb[b][:, kb, :], rhs=PT[:, c, 0:nn],
                                                            start=(kb == 0), stop=(kb == nkb - 1)),
                     reads=Vn[b] + [ptn[c]], writes=[pnO])
            if kb < ndiag0:
                if kb % 2 == 0:
                    lhold[0] = (PT, ptn)
                else:
                    PT0, ptn0 = lhold.pop(0)
                    P.op("dve", lambda e: e.tensor_tensor(out=PT0[:], in0=PT0[:], in1=PT[:], op=ALU.add),
                         reads=ptn0 + ptn, writes=ptn0)
                    lpend.append((PT0, ptn0, 0, G, kb == 1, False))
            else:
                emit_L(PT, ptn, n0, nn, False, kb == nkb - 1)
            if kb == nkb - 1:
                finalize(h, qg)

        def finalize(h, qg):
            q0 = qg * G
            T = lambda i: hT[:, i, :]
            aL0, aL1, aO0, aO1 = accL[0][0], accL[1][0], accO[0][0], accO[1][0]
            P.op("dve", lambda e: e.reciprocal(out=T(0), in_=aL0[:]), reads=[accL[0][1]], writes=["hT0"])
            P.op("dve", lambda e: e.tensor_tensor(out=T(2), in0=aO0[:], in1=T(0), op=ALU.mult),
                 reads=[accO[0][1], "hT0"], writes=["hT2"])
            P.op("dve", lambda e: e.reciprocal(out=T(1), in_=aL1[:]), reads=[accL[1][1]], writes=["hT1"])
            P.op("dve", lambda e: e.scalar_tensor_tensor(out=T(3), in0=aO1[:], scalar=dcol(DV_NLAM), in1=T(1),
                                                         op0=ALU.mult, op1=ALU.mult),
                 reads=[accO[1][1], "hT1", "derived"], writes=["hT3"])
            oi = (h * NG_OWN + qg) % 4
            P.op("dve", lambda e: e.tensor_tensor(out=xn[:, oi, :], in0=T(2), in1=T(3), op=ALU.add),
                 reads=["hT2", "hT3"], writes=[f"xn{oi}"])
            P.dma("sp", "oast", lambda e: e.dma_start(out=OAs[h][:, q0:q0 + G], in_=xn[:, oi, :]),
                  reads=[f"xn{oi}"], writes=["OAs"])

        LOOK = 1
        load_head(0)
        load_head(1)
        for i in range(len(ksteps) + LOOK):
            if i < len(ksteps):
                emit_qk(i)
            j = i - LOOK
            if j >= 0:
                emit_pv(j)
                hj = ksteps[j][0]
                if (j + 1 == len(ksteps) or ksteps[j + 1][0] != hj) and hj + 2 < 8:
                    load_head(hj + 2)

        for g in range(NG_OWN):
            o0 = g * G
            P.dma("sp", "h1ld", lambda e, o0=o0: e.dma_start(out=hT[:], in_=H1s[:, :, o0:o0 + G]),
                  reads=["H1s"], writes=N("hT", range(8)))
            P.dma("sp", "oal", lambda e, o0=o0: e.dma_start(out=xn[:], in_=OAs.rearrange("h p t -> p h t")[:, :, o0:o0 + G]),
                  reads=["OAs"], writes=N("xn", range(8)))
            P.op("act", lambda e: e.activation(out=sq[:].rearrange("p a b -> p (a b)"), in_=xn[:].rearrange("p a b -> p (a b)"),
                                               func=AF.Square), reads=N("xn", range(8)), writes=N("sq", range(8)))
            rbufs = [(ogt[:].rearrange("p a b -> p (a b)").rearrange("p (t c) -> p t c", c=512), N("ogt", range(8))),
                     (U[:].rearrange("p a b -> p (a b)").rearrange("p (t c) -> p t c", c=512), N("U", range(4)))]
            for j in range(4):
                pp = pall[:, 2 * j:2 * j + 2, :]
                ppn = [f"ps{2 * j}", f"ps{2 * j + 1}"]
                rb, rbn = rbufs[j % 2]
                for t in range(2):
                    hh = 2 * j + t
                    P.op("pe", lambda e, t=t, hh=hh, pp=pp: e.matmul(pp[:, t, :], lhsT=ones, rhs=sq[:, hh, :], start=True, stop=True),
                         reads=["cb", f"sq{hh}"], writes=[ppn[t]])
                P.op("act", lambda e, pp=pp, rb=rb: e.activation(out=rb, in_=pp, func=AF.Ln, scale=1.0 / 128, bias=EPS),
                     reads=ppn, writes=rbn)
                P.op("act", lambda e, rb=rb: e.activation(out=rb, in_=rb, func=AF.Exp, scale=-0.5), reads=rbn, writes=rbn)
                for t in range(2):
                    hh = 2 * j + t
                    P.op("dve", lambda e, t=t, hh=hh, rb=rb: e.scalar_tensor_tensor(
                        out=xn[:, hh, :], in0=xn[:, hh, :], scalar=dcol(DV_HG1), in1=rb[:, t, :], op0=ALU.mult, op1=ALU.mult),
                         reads=[f"xn{hh}", "derived"] + rbn, writes=[f"xn{hh}"])
            for blk in range(2):
                def evac_o(m, ps, pn, blk=blk):
                    mm = blk * 4 + m
                    P.op("dve", lambda e, mm=mm, ps=ps: e.tensor_tensor(out=hT[:, mm, :], in0=ps[:], in1=hT[:, mm, :], op=ALU.add),
                         reads=[pn, f"hT{mm}"], writes=[f"hT{mm}"])
                proj_fm(("b_w_out", 0, blk * 512), 4, evac_o)
            mlp_stage(1, C_MLP1)
            P.dma("sp", "out", lambda e, o0=o0: e.dma_start(out=outT[:, :, o0:o0 + G], in_=hT[:]),
                  reads=N("hT", range(8)), writes=["outT"])
        return specs

    specs = gen(Prog(nc), None)
    ids = {}
    plan = []
    for s in specs:
        if s not in ids:
            ids[s] = len(ids)
        plan.append((s, ids[s]))
    assert len(ids) <= NBLK_MAX, len(ids)
    P = Prog(nc)
    gen(P, plan)
    keys = ["out"]
    if dbg:
        keys += ["kst", "vst", "qst", "h1st", "oast"]
    P.build(final_dma_keys=keys)
    print("ops", len(P.ops), {e: len(P.eng_ops[e]) for e in ENGINES}, "waits", P.n_waits, "blocks", len(plan), len(ids))
    return nc


def make_consts():
    c = np.zeros((128, 512), np.float32)
    c[:, 0:128] = np.eye(128, dtype=np.float32)
    j = np.arange(128)[:, None]
    i = np.arange(128)[None, :]
    c[:, 128:256] = (j <= i).astype(np.float32)
    c[:, 256:384] = 1.0
    c[:, 384:512] = ((j // 64) == (i // 64)).astype(np.float32)
    return c


def make_smallp(inp, pref_bias):
    s = np.zeros((128, NSMALL), np.float32)
    fm = lambda v: np.asarray(v, np.float32).reshape(8, 128).T
    s[:, C_ANORM:C_ANORM + 8] = fm(inp["a_norm"][0])
    s[:, C_MLP0:C_MLP0 + 8] = fm(inp["mlp_norm"][0])
    s[:, C_KVN:C_KVN + 8] = fm(inp["kv_norm"])
    s[:, C_BN:C_BN + 8] = fm(inp["b_norm"][0])
    s[:, C_MLP1:C_MLP1 + 8] = fm(inp["mlp_norm"][1])
    s[:, C_AHEAD:C_AHEAD + 2] = np.asarray(inp["a_head_norm"][0], np.float32).reshape(2, 128).T
    s[:, C_KN] = np.tile(np.asarray(inp["k_norm"], np.float32), 2)
    s[:, C_QN] = np.tile(np.asarray(inp["b_q_norm"][0], np.float32), 2)
    s[:, C_BHEAD] = np.asarray(inp["b_head_norm"][0], np.float32)
    s[:, C_PREF] = pref_bias
    s[:, C_LAM:C_LAM + 256] = np.asarray(inp["b_lambda"][0], np.float32).reshape(1, 256)
    s[:, C_BGATE:C_BGATE + 512] = np.asarray(inp["a_b_gate"][0], np.float32).reshape(1, 512)
    s[:, C_KNB:C_KNB + 64] = np.asarray(inp["k_norm"], np.float32).reshape(1, 64)
    s[:, C_QNB:C_QNB + 64] = np.asarray(inp["b_q_norm"][0], np.float32).reshape(1, 64)
    return s


def make_in_maps(inp, S_ALL, S_OWN, n_batch):
    x = np.asarray(inp["x"], np.float32)
    consts = make_consts()
    shared = {
        "consts": consts,
        "a_w_in": np.ascontiguousarray(np.asarray(inp["a_w_in"], np.float32)[0]),
        "a_w_gate_up": np.ascontiguousarray(np.asarray(inp["a_w_gate_up"], np.float32)[0]),
        "a_w_out": np.ascontiguousarray(np.asarray(inp["a_w_out"], np.float32)[0]),
        "w_kv": np.ascontiguousarray(np.asarray(inp["w_kv"], np.float32)),
        "b_w_q": np.ascontiguousarray(np.asarray(inp["b_w_q"], np.float32)[0]),
        "b_w_out": np.ascontiguousarray(np.asarray(inp["b_w_out"], np.float32)[0]),
        "mlp_w1": np.ascontiguousarray(np.asarray(inp["mlp_w1"], np.float32)),
        "mlp_w2": np.ascontiguousarray(np.asarray(inp["mlp_w2"], np.float32)),
    }
    maps = []
    NPRE = S_ALL - S_OWN
    for b in range(n_batch):
        for p in range(2):
            seq = np.zeros((S_ALL, D), np.float32)
            if p == 0:
                seq[NPRE:] = x[b, 0:S_OWN]
                pref = -30000.0
            else:
                seq[:] = x[b, 0:S_ALL]
                pref = 0.0
            xTc = np.ascontiguousarray(seq.reshape(S_ALL, 8, 128).transpose(2, 1, 0))
            m = dict(shared)
            m["xT"] = xTc
            m["smallp"] = make_smallp(inp, pref)
            maps.append(m)
    return maps


_NC_CACHE = {}


def kernel(**inputs):
    S_ALL, S_OWN = 4096, 2048
    key = (S_ALL, S_OWN)
    if key not in _NC_CACHE:
        _NC_CACHE[key] = build_program(S_ALL, S_OWN)
    nc = _NC_CACHE[key]
    maps = make_in_maps(inputs, S_ALL, S_OWN, 4)
    res = run_bass_kernel_spmd(nc, maps, core_ids=list(range(8)))
    out = np.zeros((4, 4096, D), np.float32)
    for b in range(4):
        for p in range(2):
            oT = np.asarray(res.results[b * 2 + p]["outT"])
            out[b, p * S_OWN:(p + 1) * S_OWN] = oT.transpose(2, 1, 0).reshape(S_OWN, D)
    return out
```

```python
import math
import numpy as np
import concourse.bass as bass
import concourse.mybir as mybir
from concourse.bass_utils import run_bass_kernel_spmd

F32 = mybir.dt.float32
BF16 = mybir.dt.bfloat16
ALU = mybir.AluOpType
AF = mybir.ActivationFunctionType

ENGINES = ("pe", "act", "dve", "pool", "sp")
SELF_SYNC = {"pe": False, "act": True, "dve": True, "pool": True, "sp": False}
MARKS_PER_SEM = 30000


class Buf:
    __slots__ = ("name", "last_w", "readers")

    def __init__(self, name):
        self.name = name
        self.last_w = None
        self.readers = []


class Op:
    __slots__ = ("idx", "eng", "emit", "deps", "dma_key", "dma_cnt", "marked",
                 "mark_sem", "mark_val", "eidx")


class Prog:
    def __init__(self, nc):
        self.nc = nc
        self.ops = []
        self.eng_ops = {e: [] for e in ENGINES}
        self.dma_counts = {}
        self.bufs = {}

    def buf(self, name):
        b = self.bufs.get(name)
        if b is None:
            b = Buf(name)
            self.bufs[name] = b
        return b

    def _mk(self, eng, emit, reads, writes, dma_key=None):
        op = Op()
        op.idx = len(self.ops)
        op.eng = eng
        op.emit = emit
        op.dma_key = dma_key
        op.marked = False
        op.eidx = len(self.eng_ops[eng])
        deps = set()
        rb = [self.buf(r) for r in reads]
        wb = [self.buf(w) for w in writes]
        for b in rb:
            if b.last_w is not None:
                deps.add(b.last_w)
        for b in wb:
            if b.last_w is not None:
                deps.add(b.last_w)
            deps.update(b.readers)
        op.deps = deps
        if dma_key is not None:
            self.dma_counts[dma_key] = self.dma_counts.get(dma_key, 0) + 1
            op.dma_cnt = self.dma_counts[dma_key]
        for b in rb:
            b.readers.append(op.idx)
        for b in wb:
            b.last_w = op.idx
            b.readers = []
        self.ops.append(op)
        self.eng_ops[eng].append(op)
        return op

    def op(self, eng, emit, reads=(), writes=()):
        return self._mk(eng, emit, reads, writes)

    def dma(self, eng, key, emit, reads=(), writes=()):
        return self._mk(eng, emit, reads, writes, dma_key=key)

    def build(self, final_dma_keys=()):
        nc = self.nc
        ops = self.ops
        seen = {e: {p: -1 for p in ENGINES} for e in ENGINES}
        seen_dma = {e: {} for e in ENGINES}
        waits = [None] * len(ops)
        dma_issued = {}
        for op in ops:
            w_eng = {}
            w_dma = {}
            for d in op.deps:
                p = ops[d]
                if p.dma_key is not None:
                    cnt = dma_issued[p.dma_key]
                    if seen_dma[op.eng].get(p.dma_key, 0) < cnt:
                        w_dma[p.dma_key] = cnt
                else:
                    if p.eng == op.eng and not SELF_SYNC[op.eng]:
                        continue
                    if seen[op.eng][p.eng] < p.eidx:
                        if w_eng.get(p.eng, -1) < p.eidx:
                            w_eng[p.eng] = p.eidx
            for k, c in w_dma.items():
                seen_dma[op.eng][k] = c
            for pe_, ei in w_eng.items():
                seen[op.eng][pe_] = ei
                self.eng_ops[pe_][ei].marked = True
            waits[op.idx] = (w_eng, w_dma)
            if op.dma_key is not None:
                dma_issued[op.dma_key] = op.dma_cnt
        for e in ENGINES:
            n = 0
            lst = []
            for op in self.eng_ops[e]:
                if op.dma_key is None and op.marked:
                    si = n // MARKS_PER_SEM
                    if si >= len(lst):
                        lst.append(nc.alloc_semaphore(f"m_{e}_{si}"))
                    op.mark_sem = lst[si]
                    op.mark_val = n % MARKS_PER_SEM + 1
                    n += 1
        dma_sems = {k: nc.alloc_semaphore(f"d_{k}") for k in self.dma_counts}
        self.n_waits = 0
        eng_objs = {"pe": nc.tensor, "act": nc.scalar, "dve": nc.vector,
                    "pool": nc.gpsimd, "sp": nc.sync}

        def run_engine(e):
            eo = eng_objs[e]
            for op in self.eng_ops[e]:
                w_eng, w_dma = waits[op.idx]
                for pe_, ei in w_eng.items():
                    pop = self.eng_ops[pe_][ei]
                    eo.wait_ge(pop.mark_sem, pop.mark_val)
                    self.n_waits += 1
                for k, c in w_dma.items():
                    eo.wait_ge(dma_sems[k], 16 * c)
                    self.n_waits += 1
                ins = op.emit(eo)
                if op.dma_key is not None:
                    ins.then_inc(dma_sems[op.dma_key], 16)
                elif op.marked:
                    ins.then_inc(op.mark_sem, 1)
            if e == "sp":
                for k in final_dma_keys:
                    eo.wait_ge(dma_sems[k], 16 * self.dma_counts[k])

        with nc.Block() as block:
            @block.tensor
            def _(eng):
                run_engine("pe")

            @block.scalar
            def _(eng):
                run_engine("act")

            @block.vector
            def _(eng):
                run_engine("dve")

            @block.gpsimd
            def _(eng):
                run_engine("pool")

            @block.sync
            def _(eng):
                run_engine("sp")


D = 1024
KC = 8
G = 512
NCH = 4
GLA_IN = 3088
TAU = 16.0
EPS = 1e-6
LAMBDA_INIT = 0.8 - 0.6 * math.exp(-0.3 * 1)
RING = 5

C_ANORM, C_MLP0, C_KVN, C_BN, C_MLP1 = 0, 8, 16, 24, 32
C_AHEAD = 40
C_KN, C_QN, C_BHEAD, C_PREF = 42, 43, 44, 45
C_LAM = 46
C_BGATE = 302
C_KNB = 814
C_QNB = 878
NSMALL = 942


def N(prefix, it):
    return [f"{prefix}{i}" for i in it]


def build_program(S_ALL, S_OWN, dbg=False):
    NPRE = S_ALL - S_OWN
    NG_ALL = S_ALL // G
    NG_OWN = S_OWN // G
    G_OWN0 = NPRE // G
    nc = bass.Bass("TRN2", target_bir_lowering=False)

    def din(name, shape, dt=F32):
        return nc.dram_tensor(name, list(shape), dt, kind="ExternalInput").ap()

    xT = din("xT", [128, KC, S_ALL])
    smallp_d = din("smallp", [128, NSMALL])
    consts_d = din("consts", [128, 512])
    a_w_in = din("a_w_in", [D, GLA_IN])
    a_w_gate_up = din("a_w_gate_up", [16, 512])
    a_w_out = din("a_w_out", [D, D])
    w_kv = din("w_kv", [D, 2048])
    b_w_q = din("b_w_q", [D, D])
    b_w_out = din("b_w_out", [D, D])
    mlp_w1 = din("mlp_w1", [2, D, 4096])
    mlp_w2 = din("mlp_w2", [2, 4096, D])
    outT = nc.dram_tensor("outT", [128, KC, S_OWN], F32, kind="ExternalOutput").ap()

    skind = "ExternalOutput" if dbg else "Internal"
    NBLK_MAX = 48
    wstream = nc.dram_tensor("wstream", [NBLK_MAX, 128, 4096], BF16).ap()
    KTs = nc.dram_tensor("KTs", [8, 128, S_ALL], BF16, kind=skind).ap()
    Vs = nc.dram_tensor("Vs", [8, 128, S_ALL // 128, 128], BF16, kind=skind).ap()
    QTs = nc.dram_tensor("QTs", [8, 128, S_OWN], BF16, kind=skind).ap()
    H1s = nc.dram_tensor("H1s", [128, KC, S_OWN], F32, kind=skind).ap()
    OAs = nc.dram_tensor("OAs", [8, 128, S_OWN], BF16, kind=skind).ap()

    sb = nc.alloc_sbuf_tensor
    ring = [sb(f"ring{i}", [128, 8, 512], BF16) for i in range(RING)]
    hT = sb("hT", [128, KC, G], F32)
    xn = sb("xn", [128, KC, G], BF16)
    sq = sb("sq", [128, KC, G], BF16)
    big = sb("big", [128, 32, G], BF16)
    qs = sb("qs", [128, 4, G], BF16)
    ks = sb("ks", [128, 4, G], BF16)
    ktok = sb("ktok", [128, NCH, 512], BF16)
    vtok = sb("vtok", [128, NCH, 1024], BF16)
    sg = sb("sg", [128, KC, G], BF16)
    eq = sb("eq", [128, 4, G], BF16)
    ek = sb("ek", [128, 4, G], BF16)
    lg = sb("lg", [128, 512], F32)
    ogt = sb("ogt", [128, 8, 128], F32)
    sp_t = sb("sp", [128, 512], BF16)
    AT = sb("AT", [128, NCH, 4, 128], BF16)
    U = sb("U", [128, 4, 256], F32)
    Sbf = sb("Sbf", [128, NCH + 1, 4, 256], BF16)
    elast = sb("elast", [128, 4], F32)
    ecur = sb("ecur", [128, NCH, 4], F32)
    rstd = sb("rstd", [128, G], F32)
    rstd2 = sb("rstd2", [128, G], F32)
    xg = sb("xg", [128, KC, G], BF16)
    QTz = sb("QTz", [128, 2, 2, S_OWN], BF16)
    zT = sb("zT", [33, G], BF16)
    wz = sb("wz", [128, KC, 16], BF16)
    wg = sb("wg", [33, 512], BF16)
    smallp = sb("smallp_sb", [128, NSMALL], F32)
    cf = sb("cf", [128, 512], F32)
    cb = sb("cb", [128, 512], BF16)
    derived = sb("derived", [128, 16], F32)
    lamtmp = sb("lamtmp", [128, 128], F32)
    ident = cb[:, 0:128]
    tri = cb[:, 128:256]
    ones = cb[:, 256:384]
    bones = cb[:, 384:512]
    mask4 = sb("mask4", [128, 4, 128], BF16)
    pall = nc.alloc_psum_tensor("pall", [128, 8, 512], F32)
    psum = [pall[:, i, :] for i in range(8)]
    print("sbuf bytes remaining", nc.sbuf_bytes_remaining)

    DV_QG, DV_HG1, DV_NLAM, DV_BALL, DV_BPRE, DV_T0, DV_T1, DV_T2 = range(8)

    def gen(P, stream_plan):
        st = {"ps": 0, "blk": 0, "gla": 0}
        specs = []

        def PS():
            i = st["ps"] % 8
            st["ps"] += 1
            return psum[i], f"ps{i}"

        def issue_load(i):
            if stream_plan is None or i >= len(stream_plan):
                return
            slot = i % RING
            bid = stream_plan[i][1]
            P.dma("sp", f"ring{slot}",
                  lambda e, slot=slot, bid=bid: e.dma_start(
                      out=ring[slot][:].rearrange("p a b -> p (a b)"), in_=wstream[bid]),
                  reads=[f"wblk{bid}"], writes=[f"ring{slot}"])

        def next_block(spec):
            i = st["blk"]
            st["blk"] += 1
            specs.append(spec)
            if stream_plan is not None:
                assert stream_plan[i][0] == spec, (stream_plan[i], spec)
                issue_load(i + RING - 1)
            slot = i % RING
            return ring[slot], f"ring{slot}"

        def xload(g):
            t0 = g * G
            P.dma("pool", "xin", lambda e: e.dma_start(out=hT[:], in_=xT[:, :, t0:t0 + G]),
                  writes=N("hT", range(8)))

        xload(0)
        P.op("pool", lambda e: e.memset(U[:], 0.0), writes=N("U", range(4)))
        P.op("pool", lambda e: e.memset(zT[:], 0.0), writes=["zT"])
        P.op("pool", lambda e: e.memset(zT[32:33, :], 1.0), writes=["zT"])
        P.op("pool", lambda e: e.memset(wg[:], 0.0), writes=["wg"])
        P.op("pool", lambda e: e.memset(QTz[:, 0], 0.0), writes=["QTz0"])
        P.op("pool", lambda e: e.memset(QTz[:, 1], 0.0), writes=["QTz1"])
        P.op("pool", lambda e: e.memset(Sbf[:], 0.0), writes=[f"Sbf{i}_{h}" for i in range(NCH + 1) for h in range(4)])
        P.op("pool", lambda e: e.memset(elast[:], 1.0), writes=["elast"])
        if stream_plan is not None:
            P.dma("sp", "small", lambda e: e.dma_start(out=smallp[:], in_=smallp_d), writes=["smallp"])
            P.dma("sp", "small", lambda e: e.dma_start(out=cf[:], in_=consts_d), writes=["cf"])
            P.op("act", lambda e: e.activation(out=cb[:], in_=cf[:], func=AF.Copy), reads=["cf"], writes=["cb"])
            P.op("pool", lambda e: e.tensor_copy(out=mask4[:, 0, :], in_=cf[:, 128:256]), reads=["cf"], writes=["mask4"])
            for h in range(1, 4):
                P.op("pool", lambda e, h=h: e.tensor_copy(out=mask4[:, h, :], in_=cf[:, 128:256]), reads=["cf"], writes=["mask4"])

            P.dma("pool", "wz", lambda e: e.dma_start(
                out=wz[:], in_=a_w_in[:, 3072:3088].rearrange("(kc p) c -> p kc c", p=128)), writes=["wz"])
            P.dma("pool", "wz", lambda e: e.dma_start(out=wg[0:16, :], in_=a_w_gate_up), writes=["wg"])
            P.dma("pool", "wz", lambda e: e.dma_start(out=wg[32:33, :], in_=smallp_d[0:1, C_BGATE:C_BGATE + 512]), writes=["wg"])
            seen_b = set()
            for spec, bid in stream_plan:
                if bid in seen_b:
                    continue
                seen_b.add(bid)
                wname, r0, c0 = spec
                src = {"a_w_in": a_w_in, "a_w_out": a_w_out, "w_kv": w_kv, "b_w_q": b_w_q,
                       "b_w_out": b_w_out, "w1_0": mlp_w1[0], "w1_1": mlp_w1[1],
                       "w2_0": mlp_w2[0], "w2_1": mlp_w2[1]}[wname]
                sap = src[r0:r0 + 1024, c0:c0 + 512].rearrange("(kc p) c -> p kc c", p=128)
                dap = wstream[bid].rearrange("p (kc c) -> p kc c", kc=8)
                P.dma("pool", f"cast{bid}", lambda e, sap=sap, dap=dap: e.dma_start(out=dap, in_=sap),
                      writes=[f"wblk{bid}"])
            for i in range(RING - 1):
                issue_load(i)
        dcol = lambda i: derived[:, i:i + 1]
        P.op("dve", lambda e: e.tensor_scalar(out=dcol(DV_QG), in0=smallp[:, C_QN:C_QN + 1], scalar1=0.125,
                                              scalar2=None, op0=ALU.mult), reads=["smallp"], writes=["derived"])
        P.op("dve", lambda e: e.tensor_scalar(out=dcol(DV_HG1), in0=smallp[:, C_BHEAD:C_BHEAD + 1],
                                              scalar1=1.0 - LAMBDA_INIT, scalar2=None, op0=ALU.mult),
             reads=["smallp"], writes=["derived"])
        P.op("dve", lambda e: e.tensor_tensor(out=lamtmp[:, 0:64], in0=smallp[:, C_LAM:C_LAM + 64],
                                              in1=smallp[:, C_LAM + 64:C_LAM + 128], op=ALU.mult),
             reads=["smallp"], writes=["lamtmp"])
        P.op("dve", lambda e: e.tensor_tensor(out=lamtmp[:, 64:128], in0=smallp[:, C_LAM + 128:C_LAM + 192],
                                              in1=smallp[:, C_LAM + 192:C_LAM + 256], op=ALU.mult),
             reads=["smallp", "lamtmp"], writes=["lamtmp"])
        P.op("dve", lambda e: e.reduce_sum(out=dcol(DV_T0), in_=lamtmp[:, 0:64], axis=mybir.AxisListType.X),
             reads=["lamtmp", "derived"], writes=["derived"])
        P.op("dve", lambda e: e.reduce_sum(out=dcol(DV_T1), in_=lamtmp[:, 64:128], axis=mybir.AxisListType.X),
             reads=["lamtmp", "derived"], writes=["derived"])
        P.op("act", lambda e: e.activation(out=derived[:, DV_T0:DV_T1 + 1], in_=derived[:, DV_T0:DV_T1 + 1], func=AF.Exp),
             reads=["derived"], writes=["derived"])
        P.op("dve", lambda e: e.scalar_tensor_tensor(out=dcol(DV_NLAM), in0=dcol(DV_T1), scalar=-LAMBDA_INIT,
                                                     in1=dcol(DV_T0), op0=ALU.add, op1=ALU.subtract),
             reads=["derived"], writes=["derived"])
        P.op("dve", lambda e: e.tensor_reduce(out=dcol(DV_T0), in_=smallp[:, C_KNB:C_KNB + 64], axis=mybir.AxisListType.X,
                                              op=ALU.max, apply_absolute_value=True),
             reads=["smallp", "derived"], writes=["derived"])
        P.op("dve", lambda e: e.tensor_reduce(out=dcol(DV_T1), in_=smallp[:, C_QNB:C_QNB + 64], axis=mybir.AxisListType.X,
                                              op=ALU.max, apply_absolute_value=True),
             reads=["smallp", "derived"], writes=["derived"])
        P.op("dve", lambda e: e.scalar_tensor_tensor(out=dcol(DV_BALL), in0=dcol(DV_T0), scalar=-8.0,
                                                     in1=dcol(DV_T1), op0=ALU.mult, op1=ALU.mult),
             reads=["derived"], writes=["derived"])
        P.op("dve", lambda e: e.tensor_tensor(out=dcol(DV_BPRE), in0=dcol(DV_BALL), in1=smallp[:, C_PREF:C_PREF + 1],
                                              op=ALU.add), reads=["derived", "smallp"], writes=["derived"])

        def norm_stage(gcol, tag, dst=None, dstn="xn"):
            dst = xn if dst is None else dst
            P.op("act", lambda e: e.activation(out=sq[:, 0:4, :].rearrange("p a b -> p (a b)"),
                                               in_=hT[:, 0:4, :].rearrange("p a b -> p (a b)"), func=AF.Square),
                 reads=N("hT", range(0, 4)), writes=N("sq", range(0, 4)))
            P.op("dve", lambda e: e.tensor_tensor(out=sq[:, 4:8, :].rearrange("p a b -> p (a b)"),
                                                  in0=hT[:, 4:8, :].rearrange("p a b -> p (a b)"),
                                                  in1=hT[:, 4:8, :].rearrange("p a b -> p (a b)"), op=ALU.mult),
                 reads=N("hT", range(4, 8)), writes=N("sq", range(4, 8)))
            ps, pn = PS()
            for kc in range(KC):
                P.op("pe", lambda e, kc=kc, ps=ps: e.matmul(ps[:], lhsT=ones, rhs=sq[:, kc, :], start=(kc == 0), stop=(kc == KC - 1)),
                     reads=["cb", f"sq{kc}"], writes=[pn])
            P.op("act", lambda e, ps=ps: e.activation(out=rstd[:], in_=ps[:], func=AF.Ln, scale=1.0 / D, bias=EPS),
                 reads=[pn], writes=["rstd"])
            P.op("act", lambda e: e.activation(out=rstd[:], in_=rstd[:], func=AF.Exp, scale=-0.5),
                 reads=["rstd"], writes=["rstd"])
            for kc in range(KC):
                eng = "dve"
                P.op(eng, lambda e, kc=kc: e.scalar_tensor_tensor(
                    out=dst[:, kc, :], in0=hT[:, kc, :], scalar=smallp[:, gcol + kc:gcol + kc + 1], in1=rstd[:],
                    op0=ALU.mult, op1=ALU.mult),
                     reads=[f"hT{kc}", "rstd", "smallp"], writes=[f"{dstn}{kc}"])

        def proj_fm(spec, nchunks, evac, rhs_t=None, rhs_names=None):
            rt = xn if rhs_t is None else rhs_t
            rn = "xn" if rhs_names is None else rhs_names
            W, wn = next_block(spec)
            for m in range(nchunks):
                ps, pn = PS()
                for kc in range(KC):
                    P.op("pe", lambda e, kc=kc, m=m, ps=ps, W=W: e.matmul(
                        ps[:], lhsT=W[:, kc, m * 128:(m + 1) * 128], rhs=rt[:, kc, :],
                        start=(kc == 0), stop=(kc == KC - 1)),
                         reads=[wn, f"{rn}{kc}"], writes=[pn])
                evac(m, ps, pn)

        def proj_tm(spec, dst, dstname, col0, src_t=None, src_n="xn"):
            W, wn = next_block(spec)
            srct = xn if src_t is None else src_t
            for c in range(NCH):
                ps, pn = PS()
                for kc in range(KC):
                    P.op("pe", lambda e, kc=kc, c=c, ps=ps, W=W: e.matmul(
                        ps[:], lhsT=srct[:, kc, c * 128:(c + 1) * 128], rhs=W[:, kc, :],
                        start=(kc == 0), stop=(kc == KC - 1)),
                         reads=[wn, f"{src_n}{kc}"], writes=[pn])
                P.op("act", lambda e, c=c, ps=ps: e.activation(out=dst[:, c, col0:col0 + 512], in_=ps[:], func=AF.Copy),
                     reads=[pn], writes=[f"{dstname}{c}"])

        def mlp_stage(layer, ncol):
            norm_stage(ncol, "mlp")
            for b in range(8):
                def evac(m, ps, pn, b=b):
                    c = b * 4 + m
                    P.op("act", lambda e, c=c, ps=ps: e.activation(out=big[:, c, :], in_=ps[:], func=AF.Relu),
                         reads=[pn], writes=[f"hid{c}"])
                    P.op("dve", lambda e, c=c: e.tensor_tensor(out=big[:, c, :], in0=big[:, c, :], in1=big[:, c, :], op=ALU.mult),
                         reads=[f"hid{c}"], writes=[f"hid{c}"])
                proj_fm((f"w1_{layer}", 0, b * 512), 4, evac)
            for half in range(2):
                acc = [PS() for _ in range(4)]
                for kcg in range(4):
                    W, wn = next_block((f"w2_{layer}", kcg * 1024, half * 512))
                    for mm in range(4):
                        ps, pn = acc[mm]
                        for kk in range(8):
                            c = kcg * 8 + kk
                            P.op("pe", lambda e, kk=kk, mm=mm, c=c, ps=ps, W=W: e.matmul(
                                ps[:], lhsT=W[:, kk, mm * 128:(mm + 1) * 128], rhs=big[:, c, :],
                                start=(c == 0), stop=(c == 31)),
                                 reads=[wn, f"hid{c}"], writes=[pn])
                for mm in range(4):
                    ps, pn = acc[mm]
                    m = half * 4 + mm
                    P.op("dve", lambda e, m=m, ps=ps: e.tensor_tensor(out=hT[:, m, :], in0=ps[:], in1=hT[:, m, :], op=ALU.add),
                         reads=[pn, f"hT{m}"], writes=[f"hT{m}"])

        def qk_norm_stage(wname, col0, gain_ap, dst_dram, t0, key, mid=None):
            pend = []

            def chain(h, ps, pn):
                ps2, pn2 = PS()
                P.op("pe", lambda e: e.matmul(ps2[:], lhsT=bones, rhs=sq[:, h, :], start=True, stop=True),
                     reads=["cb", f"sq{h}"], writes=[pn2])
                P.op("act", lambda e: e.activation(out=rstd2[:], in_=ps2[:], func=AF.Ln, scale=1.0 / 64, bias=EPS),
                     reads=[pn2], writes=["rstd2"])
                P.op("act", lambda e: e.activation(out=rstd2[:], in_=rstd2[:], func=AF.Exp, scale=-0.5),
                     reads=["rstd2"], writes=["rstd2"])
                P.op("dve", lambda e: e.scalar_tensor_tensor(
                    out=sg[:, h, :], in0=ps[:], scalar=gain_ap, in1=rstd2[:], op0=ALU.mult, op1=ALU.mult),
                     reads=[pn, "rstd2", "smallp", "derived"], writes=[f"sg{h}"])

            for blk in range(2):
                def evac(m, ps, pn, blk=blk):
                    h = blk * 4 + m
                    P.op("act", lambda e, h=h, ps=ps: e.activation(out=sq[:, h, :], in_=ps[:], func=AF.Square),
                         reads=[pn], writes=[f"sq{h}"])
                    if pend:
                        chain(*pend.pop(0))
                    pend.append((h, ps, pn))
                proj_fm((wname, 0, col0 + blk * 512), 4, evac)
                if blk == 0 and mid is not None:
                    while pend:
                        chain(*pend.pop(0))
                    mid()
            while pend:
                chain(*pend.pop(0))
            P.dma("act", key, lambda e: e.dma_start(out=dst_dram.rearrange("h p t -> p h t")[:, :, t0:t0 + G], in_=sg[:]),
                  reads=N("sg", range(8)), writes=[key])

        def gla_norm():
            norm_stage(C_ANORM, "gla", dst=xg, dstn="xg")

        def gla_stage():
            ps, pn = PS()
            for kc in range(KC):
                P.op("pe", lambda e, kc=kc, ps=ps: e.matmul(ps[0:16, :], lhsT=wz[:, kc, :], rhs=xg[:, kc, :],
                                                              start=(kc == 0), stop=(kc == KC - 1)),
                     reads=["wz", f"xg{kc}"], writes=[pn])
            P.op("act", lambda e, ps=ps: e.activation(out=zT[0:16, :], in_=ps[0:16, :], func=AF.Copy), reads=[pn], writes=["zT"])
            for c in range(NCH):
                cs = slice(c * 128, (c + 1) * 128)
                ps, pn = PS()
                P.op("pe", lambda e, cs=cs, ps=ps: e.matmul(ps[:], lhsT=zT[:, cs], rhs=wg[:], start=True, stop=True),
                     reads=["zT", "wg"], writes=[pn])
                P.op("act", lambda e, ps=ps: e.activation(out=lg[:], in_=ps[:], func=AF.Exp, scale=-1.0), reads=[pn], writes=["lg"])
                P.op("act", lambda e: e.activation(out=sp_t[:], in_=lg[:], func=AF.Ln, bias=1.0), reads=["lg"], writes=["sp"])
                ps2, pn2 = PS()
                for h in range(4):
                    P.op("pe", lambda e, h=h, ps2=ps2: e.matmul(ps2[:, h * 128:(h + 1) * 128], lhsT=sp_t[:, h * 128:(h + 1) * 128],
                                                                 rhs=tri, start=True, stop=True),
                         reads=["sp", "cb"], writes=[pn2])
                psv = lambda p_: p_[:].rearrange("p (h i) -> p h i", h=4)
                P.op("act", lambda e, cs=cs, ps2=ps2: e.activation(out=eq[:, :, cs], in_=psv(ps2), func=AF.Exp, scale=-1.0 / TAU),
                     reads=[pn2], writes=[f"eq{c}"])
                P.op("act", lambda e, cs=cs, ps2=ps2: e.activation(out=ek[:, :, cs], in_=psv(ps2), func=AF.Exp, scale=1.0 / TAU),
                     reads=[pn2], writes=[f"ek{c}"])
            def evac_q(m, ps, pn):
                P.op("dve", lambda e, m=m, ps=ps: e.scalar_tensor_tensor(
                    out=qs[:, m, :], in0=ps[:], scalar=float(128 ** -0.5), in1=eq[:, m, :], op0=ALU.mult, op1=ALU.mult),
                     reads=[pn] + N("eq", range(4)), writes=[f"qs{m}"])
            proj_fm(("a_w_in", 0, 0), 4, evac_q, rhs_t=xg, rhs_names="xg")

            def evac_k(m, ps, pn):
                P.op("dve", lambda e, m=m, ps=ps: e.tensor_tensor(out=ks[:, m, :], in0=ps[:], in1=ek[:, m, :], op=ALU.mult),
                     reads=[pn] + N("ek", range(4)), writes=[f"ks{m}"])
            proj_fm(("a_w_in", 0, 512), 4, evac_k, rhs_t=xg, rhs_names="xg")
            proj_tm(("a_w_in", 0, 1024), vtok, "vtok", 0, src_t=xg, src_n="xg")
            proj_tm(("a_w_in", 0, 1536), vtok, "vtok", 512, src_t=xg, src_n="xg")
            for blk in range(2):
                def evac_g(m, ps, pn, blk=blk):
                    mm = blk * 4 + m
                    P.op("act", lambda e, mm=mm, ps=ps: e.activation(out=sg[:, mm, :], in_=ps[:], func=AF.Silu),
                         reads=[pn], writes=[f"sg{mm}"])
                proj_fm(("a_w_in", 0, 2048 + blk * 512), 4, evac_g, rhs_t=xg, rhs_names="xg")
            gidx = st["gla"]
            st["gla"] += 1
            CS = [slice(c * 128, (c + 1) * 128) for c in range(NCH)]
            for c in range(NCH):
                cs = CS[c]
                ps, pn = PS()
                for h in range(4):
                    P.op("pe", lambda e, h=h, cs=cs, ps=ps: e.matmul(ps[:, h * 128:(h + 1) * 128], lhsT=ks[:, h, cs], rhs=ident,
                                                                      start=True, stop=True),
                         reads=[f"ks{h}", "cb"], writes=[pn])
                P.op("act", lambda e, c=c, ps=ps: e.activation(out=ktok[:, c, :], in_=ps[:], func=AF.Copy),
                     reads=[pn], writes=[f"ktok{c}"])
                psA, pnA = PS()
                for h in range(4):
                    P.op("pe", lambda e, h=h, cs=cs, psA=psA: e.matmul(psA[:, h * 128:(h + 1) * 128], lhsT=ks[:, h, cs], rhs=qs[:, h, cs],
                                                                        start=True, stop=True),
                         reads=[f"ks{h}", f"qs{h}"], writes=[pnA])
                P.op("dve", lambda e, c=c, psA=psA: e.tensor_tensor(out=AT[:, c], in0=psA[:].rearrange("p (h i) -> p h i", h=4),
                                                                     in1=mask4[:], op=ALU.mult),
                     reads=[pnA, "mask4"], writes=[f"AT{c}"])
                P.op("act", lambda e, c=c: e.activation(out=ecur[:, c, :], in_=eq[:, :, c * 128 + 127], func=AF.Copy),
                     reads=[f"eq{c}"], writes=[f"ecur{c}"])
            for c in range(NCH):
                si_r = (gidx * NCH + c) % (NCH + 1)
                si_w = (gidx * NCH + c + 1) % (NCH + 1)
                psP = [PS(), PS()]
                for h in range(4):
                    ps_p, pn_p = psP[h // 2]
                    P.op("pe", lambda e, c=c, h=h, ps_p=ps_p: e.matmul(
                        ps_p[:, (h % 2) * 256:(h % 2 + 1) * 256], lhsT=ktok[:, c, h * 128:(h + 1) * 128],
                        rhs=vtok[:, c, h * 256:(h + 1) * 256], start=True, stop=True),
                         reads=[f"ktok{c}", f"vtok{c}"], writes=[pn_p])
                for h in range(4):
                    ps_p, pn_p = psP[h // 2]
                    ep = elast[:, h:h + 1] if c == 0 else ecur[:, c - 1, h:h + 1]
                    epn = "elast" if c == 0 else f"ecur{c - 1}"
                    P.op("dve", lambda e, h=h, ps_p=ps_p, ep=ep: e.scalar_tensor_tensor(
                        out=U[:, h, :], in0=U[:, h, :], scalar=ep, in1=ps_p[:, (h % 2) * 256:(h % 2 + 1) * 256],
                        op0=ALU.mult, op1=ALU.add),
                         reads=[f"U{h}", epn, pn_p], writes=[f"U{h}"])
                for h in range(4):
                    P.op("act", lambda e, h=h, c=c, si_w=si_w: e.activation(out=Sbf[:, si_w, h, :], in_=U[:, h, :], func=AF.Copy,
                                                                             scale=ecur[:, c, h:h + 1]),
                         reads=[f"U{h}", f"ecur{c}"], writes=[f"Sbf{si_w}_{h}"])
            P.op("dve", lambda e: e.tensor_copy(out=elast[:], in_=ecur[:, NCH - 1, :]), reads=[f"ecur{NCH - 1}"], writes=["elast"])

            psos = {}

            def emit_O(c):
                cs = CS[c]
                si_r = (gidx * NCH + c) % (NCH + 1)
                pso = [PS(), PS()]
                psos[c] = pso
                for h in range(4):
                    for vc in range(2):
                        ps_o, pn_o = pso[h // 2]
                        osl = slice(((h % 2) * 2 + vc) * 128, ((h % 2) * 2 + vc + 1) * 128)
                        vsl = slice(h * 256 + vc * 128, h * 256 + vc * 128 + 128)
                        P.op("pe", lambda e, c=c, h=h, osl=osl, vsl=vsl, ps_o=ps_o: e.matmul(
                            ps_o[:, osl], lhsT=vtok[:, c, vsl], rhs=AT[:, c, h, :], start=True, stop=False),
                             reads=[f"vtok{c}", f"AT{c}"], writes=[pn_o])
                        P.op("pe", lambda e, h=h, vc=vc, cs=cs, osl=osl, ps_o=ps_o, si_r=si_r: e.matmul(
                            ps_o[:, osl], lhsT=Sbf[:, si_r, h, vc * 128:(vc + 1) * 128], rhs=qs[:, h, cs], start=False, stop=True),
                             reads=[f"Sbf{si_r}_{h}", f"qs{h}"], writes=[pn_o])

            def emit_N(c):
                cs = CS[c]
                pso = psos.pop(c)
                psn, pnn = PS()
                for half in range(2):
                    ps_o, pn_o = pso[half]
                    P.op("act", lambda e, half=half, ps_o=ps_o: e.activation(out=sq[:, half * 4:half * 4 + 4, 0:128],
                                                                              in_=ps_o[:].rearrange("p (a i) -> p a i", a=4), func=AF.Square),
                         reads=[pn_o], writes=N("sq", range(half * 4, half * 4 + 4)))
                for h in range(4):
                    for vc in range(2):
                        P.op("pe", lambda e, h=h, vc=vc, psn=psn: e.matmul(psn[:, h * 128:(h + 1) * 128], lhsT=ones, rhs=sq[:, h * 2 + vc, 0:128],
                                                                            start=(vc == 0), stop=(vc == 1)),
                             reads=["cb", f"sq{h * 2 + vc}"], writes=[pnn])
                P.op("act", lambda e, psn=psn: e.activation(out=rstd2[:], in_=psn[:], func=AF.Ln, scale=1.0 / 256, bias=EPS),
                     reads=[pnn], writes=["rstd2"])
                P.op("act", lambda e: e.activation(out=rstd2[:], in_=rstd2[:], func=AF.Exp, scale=-0.5),
                     reads=["rstd2"], writes=["rstd2"])
                for h in range(4):
                    for vc in range(2):
                        m = h * 2 + vc
                        ps_o, pn_o = pso[h // 2]
                        osl = slice(((h % 2) * 2 + vc) * 128, ((h % 2) * 2 + vc + 1) * 128)
                        P.op("dve", lambda e, m=m, vc=vc, h=h, osl=osl, ps_o=ps_o: e.scalar_tensor_tensor(
                            out=ogt[:, m, :], in0=ps_o[:, osl], scalar=smallp[:, C_AHEAD + vc:C_AHEAD + vc + 1],
                            in1=rstd2[:, h * 128:(h + 1) * 128], op0=ALU.mult, op1=ALU.mult),
                             reads=[pn_o, "rstd2", "smallp"], writes=[f"ogt{m}"])
                        P.op("dve", lambda e, m=m, cs=cs: e.tensor_tensor(
                            out=xn[:, m, cs], in0=ogt[:, m, :], in1=sg[:, m, cs], op=ALU.mult),
                             reads=[f"ogt{m}", f"sg{m}"], writes=[f"xn{m}"])

            emit_O(0)
            for c in range(1, NCH):
                emit_O(c)
                emit_N(c - 1)
            emit_N(NCH - 1)
            for blk in range(2):
                def evac_o(m, ps, pn, blk=blk):
                    mm = blk * 4 + m
                    P.op("dve", lambda e, mm=mm, ps=ps: e.tensor_tensor(out=hT[:, mm, :], in0=ps[:], in1=hT[:, mm, :], op=ALU.add),
                         reads=[pn, f"hT{mm}"], writes=[f"hT{mm}"])
                proj_fm(("a_w_out", 0, blk * 512), 4, evac_o)

        def kv_stage(t0, after_norm=None, before_v=None):
            norm_stage(C_KVN, "kv")
            if after_norm is not None:
                after_norm()
            qk_norm_stage("w_kv", 0, smallp[:, C_KN:C_KN + 1], KTs, t0, "kst")
            if before_v is not None:
                before_v()
            proj_tm(("w_kv", 0, 1024), vtok, "vtok", 0)
            proj_tm(("w_kv", 0, 1536), vtok, "vtok", 512)
            blk0 = t0 // 128
            for c in range(NCH):
                P.dma("act", "vst", lambda e, c=c: e.dma_start(
                    out=Vs.rearrange("h p b v -> p b h v")[:, blk0 + c, :, :],
                    in_=vtok[:, c, :].rearrange("p (h v) -> p h v", h=8)),
                      reads=[f"vtok{c}"], writes=["Vs"])

        gla_norm()
        for g in range(NG_ALL):
            t0 = g * G
            gla_stage()
            mlp_stage(0, C_MLP0)
            has_next = g + 1 < NG_ALL
            nxt = (lambda g=g: xload(g + 1)) if has_next else None
            pre = gla_norm if has_next else None
            if g < G_OWN0:
                kv_stage(t0, after_norm=nxt, before_v=pre)
            else:
                kv_stage(t0)
                o0 = t0 - NPRE
                P.dma("act", "h1st", lambda e, o0=o0: e.dma_start(out=H1s[:, :, o0:o0 + G], in_=hT[:]),
                      reads=N("hT", range(8)), writes=["H1s"])
                for kc in range(KC):
                    P.op("dve", lambda e, kc=kc: e.scalar_tensor_tensor(
                        out=xn[:, kc, :], in0=hT[:, kc, :], scalar=smallp[:, C_BN + kc:C_BN + kc + 1], in1=rstd[:],
                        op0=ALU.mult, op1=ALU.mult),
                         reads=[f"hT{kc}", "rstd", "smallp"], writes=[f"xn{kc}"])
                if nxt is not None:
                    nxt()
                qk_norm_stage("b_w_q", 0, dcol(DV_QG), QTs, o0, "qst", mid=pre)

        KTb = [big[:, 0:8, :].rearrange("p a b -> p (a b)"), big[:, 8:16, :].rearrange("p a b -> p (a b)")]
        KTn = [N("hid", range(0, 8)), N("hid", range(8, 16))]
        Vb = [big[:, 16:24, :].rearrange("p a (b v) -> p (a b) v", v=128), big[:, 24:32, :].rearrange("p a (b v) -> p (a b) v", v=128)]
        Vn = [N("hid", range(16, 24)), N("hid", range(24, 32))]
        QTn = ["QTz0", "QTz1"]
        NKB_ALL = S_ALL // 128
        NPB = NPRE // 128
        LOOK = 2

        def load_head(h):
            b = h % 2
            P.dma("sp", f"ktl{b}", lambda e: e.dma_start(out=KTb[b][:, 0:S_ALL], in_=KTs[h]),
                  reads=["kst"], writes=KTn[b])
            P.dma("sp", f"vl{b}", lambda e: e.dma_start(out=Vb[b][:, 0:NKB_ALL, :], in_=Vs[h]),
                  reads=["Vs"], writes=Vn[b])
            P.dma("sp", f"ql{b}", lambda e: e.dma_start(out=QTz[0:64, b, 0, 0:S_OWN], in_=QTs[h][0:64, :]),
                  reads=["qst"], writes=[QTn[b]])
            P.dma("sp", f"ql{b}", lambda e: e.dma_start(out=QTz[64:128, b, 1, 0:S_OWN], in_=QTs[h][64:128, :]),
                  reads=["qst"], writes=[QTn[b]])

        ksteps = []
        for h in range(8):
            for qg in range(NG_OWN):
                nkb = NPB + (qg + 1) * 4
                for kb in range(nkb):
                    ksteps.append((h, qg, kb, nkb))
        rotp = [0]
        accO = [(psum[0], "ps0"), (psum[1], "ps1")]
        accL = [(psum[2], "ps2"), (psum[3], "ps3")]
        pts = {}

        def emit_qk(i):
            h, qg, kb, nkb = ksteps[i]
            b = h % 2
            q0 = qg * G
            dg = kb - (NPB + qg * 4)
            n0 = dg * 128 if dg > 0 else 0
            nn = G - n0
            k = rotp[0] % 2
            rotp[0] += 1
            base = 4 + 2 * k
            pp = pall[:, base:base + 2, :]
            ppn = [f"ps{base}", f"ps{base + 1}"]
            for c in range(2):
                P.op("pe", lambda e, c=c: e.matmul(pp[:, c, 0:nn], lhsT=KTb[b][:, kb * 128:(kb + 1) * 128],
                                                   rhs=QTz[:, b, c, q0 + n0:q0 + G], start=True, stop=True),
                     reads=KTn[b] + [QTn[b]], writes=[ppn[c]])
            pi = i % 4
            PT = sg[:, 2 * pi:2 * pi + 2, :]
            ptn = [f"sg{2 * pi}", f"sg{2 * pi + 1}"]
            bias_ap = dcol(DV_BPRE) if kb < NPB else dcol(DV_BALL)
            P.op("act", lambda e: e.activation(out=PT[:, :, 0:nn], in_=pp[:, :, 0:nn], func=AF.Exp, bias=bias_ap),
                 reads=ppn + ["derived"], writes=ptn)
            if dg >= 0:
                P.op("dve", lambda e: e.tensor_tensor(out=PT[:, :, 0:128], in0=PT[:, :, 0:128], in1=mask4[:, 0:2, :], op=ALU.mult),
                     reads=ptn + ["mask4"], writes=ptn)
            pts[i] = (PT, ptn, n0, nn)

        lpend = []
        lhold = {}

        def emit_L(PTs, ptn, n0, nn, first, last):
            for c in range(2):
                psL, pnL = accL[c]
                P.op("pe", lambda e, c=c, psL=psL: e.matmul(psL[:, n0:G], lhsT=ones, rhs=PTs[:, c, 0:nn], start=first, stop=last),
                     reads=["cb", ptn[c]], writes=[pnL])

        def emit_pv(i):
            h, qg, kb, nkb = ksteps[i]
            b = h % 2
            PT, ptn, n0, nn = pts.pop(i)
            ndiag0 = NPB + qg * 4
            while lpend:
                emit_L(*lpend.pop(0))
            for c in range(2):
                psO, pnO = accO[c]
                P.op("pe", lambda e, c=c, psO=psO: e.matmul(psO[:, n0:G], lhsT=Vb[b][:, kb, :], rhs=PT[:, c, 0:nn],
                                                            start=(kb == 0), stop=(kb == nkb - 1)),
                     reads=Vn[b] + [ptn[c]], writes=[pnO])
            if kb < ndiag0:
                if kb % 2 == 0:
                    lhold[0] = (PT, ptn)
                else:
                    PT0, ptn0 = lhold.pop(0)
                    P.op("dve", lambda e: e.tensor_tensor(out=PT0[:], in0=PT0[:], in1=PT[:], op=ALU.add),
                         reads=ptn0 + ptn, writes=ptn0)
                    lpend.append((PT0, ptn0, 0, G, kb == 1, False))
            else:
                emit_L(PT, ptn, n0, nn, False, kb == nkb - 1)
            if kb == nkb - 1:
                finalize(h, qg)

        def finalize(h, qg):
            q0 = qg * G
            T = lambda i: hT[:, i, :]
            aL0, aL1, aO0, aO1 = accL[0][0], accL[1][0], accO[0][0], accO[1][0]
            P.op("dve", lambda e: e.reciprocal(out=T(0), in_=aL0[:]), reads=[accL[0][1]], writes=["hT0"])
            P.op("dve", lambda e: e.tensor_tensor(out=T(2), in0=aO0[:], in1=T(0), op=ALU.mult),
                 reads=[accO[0][1], "hT0"], writes=["hT2"])
            P.op("dve", lambda e: e.reciprocal(out=T(1), in_=aL1[:]), reads=[accL[1][1]], writes=["hT1"])
            P.op("dve", lambda e: e.scalar_tensor_tensor(out=T(3), in0=aO1[:], scalar=dcol(DV_NLAM), in1=T(1),
                                                         op0=ALU.mult, op1=ALU.mult),
                 reads=[accO[1][1], "hT1", "derived"], writes=["hT3"])
            oi = (h * NG_OWN + qg) % 4
            P.op("dve", lambda e: e.tensor_tensor(out=xn[:, oi, :], in0=T(2), in1=T(3), op=ALU.add),
                 reads=["hT2", "hT3"], writes=[f"xn{oi}"])
            P.dma("sp", "oast", lambda e: e.dma_start(out=OAs[h][:, q0:q0 + G], in_=xn[:, oi, :]),
                  reads=[f"xn{oi}"], writes=["OAs"])

        LOOK = 1
        load_head(0)
        load_head(1)
        for i in range(len(ksteps) + LOOK):
            if i < len(ksteps):
                emit_qk(i)
            j = i - LOOK
            if j >= 0:
                emit_pv(j)
                hj = ksteps[j][0]
                if (j + 1 == len(ksteps) or ksteps[j + 1][0] != hj) and hj + 2 < 8:
                    load_head(hj + 2)

        for g in range(NG_OWN):
            o0 = g * G
            P.dma("sp", "h1ld", lambda e, o0=o0: e.dma_start(out=hT[:], in_=H1s[:, :, o0:o0 + G]),
                  reads=["H1s"], writes=N("hT", range(8)))
            P.dma("sp", "oal", lambda e, o0=o0: e.dma_start(out=xn[:], in_=OAs.rearrange("h p t -> p h t")[:, :, o0:o0 + G]),
                  reads=["OAs"], writes=N("xn", range(8)))
            P.op("act", lambda e: e.activation(out=sq[:].rearrange("p a b -> p (a b)"), in_=xn[:].rearrange("p a b -> p (a b)"),
                                               func=AF.Square), reads=N("xn", range(8)), writes=N("sq", range(8)))
            rbufs = [(ogt[:].rearrange("p a b -> p (a b)").rearrange("p (t c) -> p t c", c=512), N("ogt", range(8))),
                     (U[:].rearrange("p a b -> p (a b)").rearrange("p (t c) -> p t c", c=512), N("U", range(4)))]
            for j in range(4):
                pp = pall[:, 2 * j:2 * j + 2, :]
                ppn = [f"ps{2 * j}", f"ps{2 * j + 1}"]
                rb, rbn = rbufs[j % 2]
                for t in range(2):
                    hh = 2 * j + t
                    P.op("pe", lambda e, t=t, hh=hh, pp=pp: e.matmul(pp[:, t, :], lhsT=ones, rhs=sq[:, hh, :], start=True, stop=True),
                         reads=["cb", f"sq{hh}"], writes=[ppn[t]])
                P.op("act", lambda e, pp=pp, rb=rb: e.activation(out=rb, in_=pp, func=AF.Ln, scale=1.0 / 128, bias=EPS),
                     reads=ppn, writes=rbn)
                P.op("act", lambda e, rb=rb: e.activation(out=rb, in_=rb, func=AF.Exp, scale=-0.5), reads=rbn, writes=rbn)
                for t in range(2):
                    hh = 2 * j + t
                    P.op("dve", lambda e, t=t, hh=hh, rb=rb: e.scalar_tensor_tensor(
                        out=xn[:, hh, :], in0=xn[:, hh, :], scalar=dcol(DV_HG1), in1=rb[:, t, :], op0=ALU.mult, op1=ALU.mult),
                         reads=[f"xn{hh}", "derived"] + rbn, writes=[f"xn{hh}"])
            for blk in range(2):
                def evac_o(m, ps, pn, blk=blk):
                    mm = blk * 4 + m
                    P.op("dve", lambda e, mm=mm, ps=ps: e.tensor_tensor(out=hT[:, mm, :], in0=ps[:], in1=hT[:, mm, :], op=ALU.add),
                         reads=[pn, f"hT{mm}"], writes=[f"hT{mm}"])
                proj_fm(("b_w_out", 0, blk * 512), 4, evac_o)
            mlp_stage(1, C_MLP1)
            P.dma("sp", "out", lambda e, o0=o0: e.dma_start(out=outT[:, :, o0:o0 + G], in_=hT[:]),
                  reads=N("hT", range(8)), writes=["outT"])
        return specs

    specs = gen(Prog(nc), None)
    ids = {}
    plan = []
    for s in specs:
        if s not in ids:
            ids[s] = len(ids)
        plan.append((s, ids[s]))
    assert len(ids) <= NBLK_MAX, len(ids)
    P = Prog(nc)
    gen(P, plan)
    keys = ["out"]
    if dbg:
        keys += ["kst", "vst", "qst", "h1st", "oast"]
    P.build(final_dma_keys=keys)
    print("ops", len(P.ops), {e: len(P.eng_ops[e]) for e in ENGINES}, "waits", P.n_waits, "blocks", len(plan), len(ids))
    return nc


def make_consts():
    c = np.zeros((128, 512), np.float32)
    c[:, 0:128] = np.eye(128, dtype=np.float32)
    j = np.arange(128)[:, None]
    i = np.arange(128)[None, :]
    c[:, 128:256] = (j <= i).astype(np.float32)
    c[:, 256:384] = 1.0
    c[:, 384:512] = ((j // 64) == (i // 64)).astype(np.float32)
    return c


def make_smallp(inp, pref_bias):
    s = np.zeros((128, NSMALL), np.float32)
    fm = lambda v: np.asarray(v, np.float32).reshape(8, 128).T
    s[:, C_ANORM:C_ANORM + 8] = fm(inp["a_norm"][0])
    s[:, C_MLP0:C_MLP0 + 8] = fm(inp["mlp_norm"][0])
    s[:, C_KVN:C_KVN + 8] = fm(inp["kv_norm"])
    s[:, C_BN:C_BN + 8] = fm(inp["b_norm"][0])
    s[:, C_MLP1:C_MLP1 + 8] = fm(inp["mlp_norm"][1])
    s[:, C_AHEAD:C_AHEAD + 2] = np.asarray(inp["a_head_norm"][0], np.float32).reshape(2, 128).T
    s[:, C_KN] = np.tile(np.asarray(inp["k_norm"], np.float32), 2)
    s[:, C_QN] = np.tile(np.asarray(inp["b_q_norm"][0], np.float32), 2)
    s[:, C_BHEAD] = np.asarray(inp["b_head_norm"][0], np.float32)
    s[:, C_PREF] = pref_bias
    s[:, C_LAM:C_LAM + 256] = np.asarray(inp["b_lambda"][0], np.float32).reshape(1, 256)
    s[:, C_BGATE:C_BGATE + 512] = np.asarray(inp["a_b_gate"][0], np.float32).reshape(1, 512)
    s[:, C_KNB:C_KNB + 64] = np.asarray(inp["k_norm"], np.float32).reshape(1, 64)
    s[:, C_QNB:C_QNB + 64] = np.asarray(inp["b_q_norm"][0], np.float32).reshape(1, 64)
    return s


def make_in_maps(inp, S_ALL, S_OWN, n_batch):
    x = np.asarray(inp["x"], np.float32)
    consts = make_consts()
    shared = {
        "consts": consts,
        "a_w_in": np.ascontiguousarray(np.asarray(inp["a_w_in"], np.float32)[0]),
        "a_w_gate_up": np.ascontiguousarray(np.asarray(inp["a_w_gate_up"], np.float32)[0]),
        "a_w_out": np.ascontiguousarray(np.asarray(inp["a_w_out"], np.float32)[0]),
        "w_kv": np.ascontiguousarray(np.asarray(inp["w_kv"], np.float32)),
        "b_w_q": np.ascontiguousarray(np.asarray(inp["b_w_q"], np.float32)[0]),
        "b_w_out": np.ascontiguousarray(np.asarray(inp["b_w_out"], np.float32)[0]),
        "mlp_w1": np.ascontiguousarray(np.asarray(inp["mlp_w1"], np.float32)),
        "mlp_w2": np.ascontiguousarray(np.asarray(inp["mlp_w2"], np.float32)),
    }
    maps = []
    NPRE = S_ALL - S_OWN
    for b in range(n_batch):
        for p in range(2):
            seq = np.zeros((S_ALL, D), np.float32)
            if p == 0:
                seq[NPRE:] = x[b, 0:S_OWN]
                pref = -30000.0
            else:
                seq[:] = x[b, 0:S_ALL]
                pref = 0.0
            xTc = np.ascontiguousarray(seq.reshape(S_ALL, 8, 128).transpose(2, 1, 0))
            m = dict(shared)
            m["xT"] = xTc
            m["smallp"] = make_smallp(inp, pref)
            maps.append(m)
    return maps


_NC_CACHE = {}


def kernel(**inputs):
    S_ALL, S_OWN = 4096, 2048
    key = (S_ALL, S_OWN)
    if key not in _NC_CACHE:
        _NC_CACHE[key] = build_program(S_ALL, S_OWN)
    nc = _NC_CACHE[key]
    maps = make_in_maps(inputs, S_ALL, S_OWN, 4)
    res = run_bass_kernel_spmd(nc, maps, core_ids=list(range(8)))
    out = np.zeros((4, 4096, D), np.float32)
    for b in range(4):
        for p in range(2):
            oT = np.asarray(res.results[b * 2 + p]["outT"])
            out[b, p * S_OWN:(p + 1) * S_OWN] = oT.transpose(2, 1, 0).reshape(S_OWN, D)
    return out
```
